# Optimizing a Trainium2 kernel written in Bass

```python
import math
import jax, jax.numpy as jnp
from jax import lax
import numpy as np

D_MODEL = 1024
BATCH = 16
SEQ = 2048
DEPTH = 1

MIX_WIDTH = D_MODEL
ATTN_WIDTH = MIX_WIDTH // 2
POOL_WIDTH = MIX_WIDTH - ATTN_WIDTH
N_DIFF_HEADS = 4
DIFF_HEAD_DIM = ATTN_WIDTH // (2 * N_DIFF_HEADS)
DIFF_VALUE_DIM = 2 * DIFF_HEAD_DIM
ROT_DIM = DIFF_HEAD_DIM // 4
ROPE_THETA = 500000.0
Q_BLOCK = 128
POOL_WINDOWS = (2, 4, 8, 16)
N_POOL_GROUPS = len(POOL_WINDOWS)
POOL_GROUP_DIM = POOL_WIDTH // N_POOL_GROUPS
IN_COLS = 3 * ATTN_WIDTH + POOL_WIDTH
N_EXPERT_GROUPS = 4
EXPERTS_PER_GROUP = 8
N_EXPERTS = N_EXPERT_GROUPS * EXPERTS_PER_GROUP
TOP_K_IN_GROUP = 2
D_EXPERT = D_MODEL // 2
MOE_BLOCK = 128
RMS_EPS = 1e-6
N_MOD = 6

kernel_name = "hybrid_diffattn_pool_hmoe_encoder"


def rms_norm(x, g):
    xf = x.astype(jnp.float32)
    y = xf * lax.rsqrt(jnp.mean(xf * xf, axis=-1, keepdims=True) + RMS_EPS)
    return (y * g.astype(jnp.float32)).astype(x.dtype)


def lambda_init_for(layer_idx):
    return 0.8 - 0.6 * math.exp(-0.3 * layer_idx)


def rope_tables(seq_len):
    pos = jnp.arange(seq_len, dtype=jnp.float32)
    inv_freq = ROPE_THETA ** (-jnp.arange(0, ROT_DIM, 2, dtype=jnp.float32) / ROT_DIM)
    ang = pos[:, None] * inv_freq[None, :]
    return jnp.cos(ang), jnp.sin(ang)


def apply_partial_rope(t, cos, sin):
    half = ROT_DIM // 2
    cs = cos[None, :, None, None, :].astype(t.dtype)
    sn = sin[None, :, None, None, :].astype(t.dtype)
    t1 = t[..., :half]
    t2 = t[..., half:ROT_DIM]
    return jnp.concatenate([t1 * cs - t2 * sn, t2 * cs + t1 * sn, t[..., ROT_DIM:]], axis=-1)


def diff_attention(q, k, v, lam, subln_g, lambda_init):
    B, S = q.shape[0], q.shape[1]
    cos, sin = rope_tables(S)
    q = apply_partial_rope(q, cos, sin) * (DIFF_HEAD_DIM ** -0.5)
    k = apply_partial_rope(k, cos, sin)
    nqb = S // Q_BLOCK
    qb = q.reshape(B, nqb, Q_BLOCK, N_DIFF_HEADS, 2, DIFF_HEAD_DIM).transpose(1, 0, 3, 4, 2, 5)
    kt = k.transpose(0, 2, 3, 1, 4)
    vt = v.transpose(0, 2, 1, 3)

    def block(qblk):
        s = jnp.einsum('bhcqd,bhckd->bhcqk', qblk, kt).astype(jnp.float32)
        a = jax.nn.softmax(s, axis=-1)
        w = a[:, :, 0] - lam * a[:, :, 1]
        return jnp.einsum('bhqk,bhkv->bhqv', w.astype(vt.dtype), vt)

    o = lax.map(block, qb)
    o = o.transpose(1, 0, 3, 2, 4).reshape(B, S, N_DIFF_HEADS, DIFF_VALUE_DIM)
    o = rms_norm(o, subln_g) * (1.0 - lambda_init)
    return o.reshape(B, S, N_DIFF_HEADS * DIFF_VALUE_DIM)


def multiscale_pool(u, w_pool, pool_scale):
    B, S = u.shape[0], u.shape[1]
    uf = u.astype(jnp.float32)
    cs = jnp.concatenate([jnp.zeros_like(uf[:, :1]), jnp.cumsum(uf, axis=1)], axis=1)
    pos = jnp.arange(S)
    outs = []
    for g, w in enumerate(POOL_WINDOWS):
        left = w // 2
        right = w - 1 - left
        lo = jnp.clip(pos - left, 0, S)
        hi = jnp.clip(pos + right + 1, 0, S)
        cnt = (hi - lo).astype(jnp.float32)
        mean = (cs[:, hi, g] - cs[:, lo, g]) / cnt[None, :, None]
        outs.append(mean - uf[:, :, g])
    d = jnp.stack(outs, axis=2).astype(u.dtype)
    y = jnp.einsum('bsgc,gcd->bsgd', d, w_pool) * pool_scale
    return y.reshape(B, S, N_POOL_GROUPS * POOL_GROUP_DIM)


def grouped_expert_ffn(xt, flat_e, flat_tok, flat_w, w_gate, w_up, w_down):
    N, D = xt.shape
    NK = flat_e.shape[0]
    order = jnp.argsort(flat_e)
    sorted_e = flat_e[order]
    counts = jax.ops.segment_sum(jnp.ones((NK,), jnp.int32), flat_e, num_segments=N_EXPERTS)
    starts = jnp.cumsum(counts) - counts
    padded = ((counts + MOE_BLOCK - 1) // MOE_BLOCK) * MOE_BLOCK
    pad_ends = jnp.cumsum(padded)
    pad_starts = pad_ends - padded
    rank = jnp.arange(NK, dtype=jnp.int32) - starts[sorted_e]
    dest = pad_starts[sorted_e] + rank
    n_blocks = -(-NK // MOE_BLOCK) + N_EXPERTS
    P = n_blocks * MOE_BLOCK
    row_tok = jnp.full((P,), N, jnp.int32).at[dest].set(flat_tok[order])
    row_w = jnp.zeros((P,), jnp.float32).at[dest].set(flat_w[order])
    block_e = jnp.minimum(
        jnp.searchsorted(pad_ends, jnp.arange(n_blocks, dtype=jnp.int32) * MOE_BLOCK, side='right'),
        N_EXPERTS - 1).astype(jnp.int32)
    x_pad = jnp.concatenate([xt, jnp.zeros((1, D), xt.dtype)], axis=0)
    xb = x_pad[row_tok].reshape(n_blocks, MOE_BLOCK, D)

    def expert_block(args):
        xblk, e = args
        hid = jax.nn.silu(xblk @ w_gate[e]) * (xblk @ w_up[e])
        return hid @ w_down[e]

    yb = lax.map(expert_block, (xb, block_e)).reshape(P, D)
    y = jnp.zeros((N + 1, D), jnp.float32).at[row_tok].add(yb.astype(jnp.float32) * row_w[:, None])
    return y[:N].astype(xt.dtype)


def hierarchical_moe(h, w_rg, b_rg, w_re, b_re, w_gate, w_up, w_down):
    B, S, D = h.shape
    N = B * S
    xt = h.reshape(N, D)
    g_logits = (xt @ w_rg).astype(jnp.float32) + b_rg.astype(jnp.float32)
    g_prob = jax.nn.softmax(g_logits, axis=-1)
    _, g_idx = lax.top_k(g_logits, 1)
    g_sel = g_idx[:, 0]
    tok = jnp.arange(N)
    g_p = g_prob[tok, g_sel]
    e_logits = ((xt @ w_re).astype(jnp.float32) + b_re.astype(jnp.float32)).reshape(N, N_EXPERT_GROUPS, EXPERTS_PER_GROUP)
    e_in = e_logits[tok, g_sel]
    e_prob = jax.nn.softmax(e_in, axis=-1)
    top_p, top_i = lax.top_k(e_prob, TOP_K_IN_GROUP)
    top_p = top_p / jnp.sum(top_p, axis=-1, keepdims=True)
    weights = g_p[:, None] * top_p
    expert_id = (g_sel[:, None] * EXPERTS_PER_GROUP + top_i).astype(jnp.int32)
    flat_tok = jnp.repeat(jnp.arange(N, dtype=jnp.int32), TOP_K_IN_GROUP)
    y = grouped_expert_ffn(xt, expert_id.reshape(-1), flat_tok, weights.reshape(-1), w_gate, w_up, w_down)
    return y.reshape(B, S, D)


def setup_inputs(seed: int = 0) -> dict:
    key = jax.random.key(seed)
    ks = jax.random.split(key, 24)
    f32 = jnp.float32
    D = D_MODEL
    nrm = lambda k, shape, s: jax.random.normal(k, shape, f32) * s
    return {
        "x": nrm(ks[0], (BATCH, SEQ, D), 1.0),
        "c": nrm(ks[1], (BATCH, D), 1.0),
        "w_ada": nrm(ks[2], (DEPTH, D, N_MOD * D), 0.3 * D ** -0.5),
        "b_ada": nrm(ks[3], (DEPTH, N_MOD * D), 0.02),
        "norm1_g": 1.0 + nrm(ks[4], (DEPTH, D), 0.02),
        "w_in": nrm(ks[5], (DEPTH, D, IN_COLS), D ** -0.5),
        "lambda_q1": nrm(ks[6], (DEPTH, DIFF_HEAD_DIM), 0.1),
        "lambda_k1": nrm(ks[7], (DEPTH, DIFF_HEAD_DIM), 0.1),
        "lambda_q2": nrm(ks[8], (DEPTH, DIFF_HEAD_DIM), 0.1),
        "lambda_k2": nrm(ks[9], (DEPTH, DIFF_HEAD_DIM), 0.1),
        "subln_g": 1.0 + nrm(ks[10], (DEPTH, DIFF_VALUE_DIM), 0.02),
        "w_pool": nrm(ks[11], (DEPTH, N_POOL_GROUPS, POOL_GROUP_DIM, POOL_GROUP_DIM), POOL_GROUP_DIM ** -0.5),
        "pool_scale": 1.0 + nrm(ks[12], (DEPTH, N_POOL_GROUPS, POOL_GROUP_DIM), 0.1),
        "w_out": nrm(ks[13], (DEPTH, MIX_WIDTH, D), MIX_WIDTH ** -0.5),
        "norm2_g": 1.0 + nrm(ks[14], (DEPTH, D), 0.02),
        "w_router_group": nrm(ks[15], (DEPTH, D, N_EXPERT_GROUPS), D ** -0.5),
        "b_router_group": nrm(ks[16], (DEPTH, N_EXPERT_GROUPS), 0.01),
        "w_router_expert": nrm(ks[17], (DEPTH, D, N_EXPERTS), D ** -0.5),
        "b_router_expert": nrm(ks[18], (DEPTH, N_EXPERTS), 0.01),
        "w_gate": nrm(ks[19], (DEPTH, N_EXPERTS, D, D_EXPERT), D ** -0.5),
        "w_up": nrm(ks[20], (DEPTH, N_EXPERTS, D, D_EXPERT), D ** -0.5),
        "w_down": nrm(ks[21], (DEPTH, N_EXPERTS, D_EXPERT, D), D_EXPERT ** -0.5),
        "final_g": 1.0 + nrm(ks[22], (D,), 0.02),
    }


def reference(x, c, w_ada, b_ada, norm1_g, w_in, lambda_q1, lambda_k1, lambda_q2, lambda_k2,
              subln_g, w_pool, pool_scale, w_out, norm2_g, w_router_group, b_router_group,
              w_router_expert, b_router_expert, w_gate, w_up, w_down, final_g):
    B, S, D = x.shape
    A = ATTN_WIDTH
    for l in range(DEPTH):
        lambda_init = lambda_init_for(l)
        mod = jax.nn.silu(c) @ w_ada[l] + b_ada[l]
        sh1, sc1, g1, sh2, sc2, g2 = jnp.split(mod, N_MOD, axis=-1)
        h = rms_norm(x, norm1_g[l]) * (1.0 + sc1[:, None]) + sh1[:, None]
        z = h @ w_in[l]
        q = z[..., :A].reshape(B, S, N_DIFF_HEADS, 2, DIFF_HEAD_DIM)
        k = z[..., A:2 * A].reshape(B, S, N_DIFF_HEADS, 2, DIFF_HEAD_DIM)
        v = z[..., 2 * A:3 * A].reshape(B, S, N_DIFF_HEADS, DIFF_VALUE_DIM)
        u = z[..., 3 * A:].reshape(B, S, N_POOL_GROUPS, POOL_GROUP_DIM)
        lam = (jnp.exp(jnp.sum(lambda_q1[l].astype(jnp.float32) * lambda_k1[l].astype(jnp.float32)))
               - jnp.exp(jnp.sum(lambda_q2[l].astype(jnp.float32) * lambda_k2[l].astype(jnp.float32)))
               + lambda_init)
        a_out = diff_attention(q, k, v, lam, subln_g[l], lambda_init)
        p_out = multiscale_pool(u, w_pool[l], pool_scale[l])
        mix = jnp.concatenate([a_out, p_out], axis=-1) @ w_out[l]
        x = x + g1[:, None] * mix
        h = rms_norm(x, norm2_g[l]) * (1.0 + sc2[:, None]) + sh2[:, None]
        y = hierarchical_moe(h, w_router_group[l], b_router_group[l], w_router_expert[l],
                             b_router_expert[l], w_gate[l], w_up[l], w_down[l])
        x = x + g2[:, None] * y
    return rms_norm(x, final_g)
```

```python
import math
import numpy as np
import concourse.bass as bass
import concourse.mybir as mybir
from concourse.bass_utils import run_bass_kernel_spmd

F32 = mybir.dt.float32
BF16 = mybir.dt.bfloat16
AF = mybir.ActivationFunctionType
ALU = mybir.AluOpType
AX = mybir.AxisListType

D = 1024
KT = 8
NCORES = 8
N_MOD = 6
NH = 4
NG = 4
EPG = 8
NE = 32
DE = 512
RMS_EPS = 1e-6
LAMBDA_INIT = 0.8 - 0.6 * math.exp(-0.3 * 0)
ROPE_THETA = 500000.0
POOL_WINDOWS = (2, 4, 8, 16)


class Sem:
    def __init__(self, handle):
        self.handle = handle
        self.count = 0


class Cell:
    __slots__ = ("w", "r", "excl", "name")

    def __init__(self, name="", excl=False):
        self.w = None
        self.r = {}
        self.excl = excl
        self.name = name


class Eng:
    def __init__(self, name, h, sem):
        self.name = name
        self.h = h
        self.sem = sem
        self.seen = {}


class Prog:
    def __init__(self, nc):
        self.nc = nc
        self.cells = []
        self.eng = {}
        for name, h in (("pe", nc.tensor), ("act", nc.scalar), ("dve", nc.vector),
                        ("pool", nc.gpsimd), ("sp", nc.sync)):
            sem = Sem(nc.alloc_semaphore("s_" + name)) if name != "sp" else None
            self.eng[name] = Eng(name, h, sem)
        self.dsem = {}
        self.ninst = 0

    def cell(self, name="", excl=False):
        c = Cell(name, excl)
        self.cells.append(c)
        return c

    def _deps(self, e, reads, writes):
        deps = {}

        def need(s, v):
            if deps.get(s, 0) < v:
                deps[s] = v

        for c in reads:
            if c.w is not None:
                need(*c.w)
            if c.excl:
                for s, v in c.r.items():
                    need(s, v)
        for c in writes:
            if c.w is not None:
                need(*c.w)
            for s, v in c.r.items():
                need(s, v)
        for s, v in deps.items():
            if e.name == "pe" and s is e.sem:
                continue
            if e.seen.get(s, 0) >= v:
                continue
            e.h.wait_ge(s.handle, v)
            e.seen[s] = v
            self.ninst += 1

    def _mark(self, tok, reads, writes):
        s, v = tok
        for c in reads:
            if c.excl:
                c.w = tok
                c.r = {}
            else:
                if c.r.get(s, 0) < v:
                    c.r[s] = v
        for c in writes:
            c.w = tok
            c.r = {}

    def op(self, en, fn, reads=(), writes=(), inc=True):
        e = self.eng[en]
        self._deps(e, reads, writes)
        ins = fn(e.h)
        self.ninst += 1
        if inc:
            e.sem.count += 1
            ins.then_inc(e.sem.handle, 1)
            tok = (e.sem, e.sem.count)
        else:
            tok = (e.sem, e.sem.count + 1)
        self._mark(tok, reads, writes)
        return ins

    def dma(self, q, out, in_, reads=(), writes=(), key=None):
        e = self.eng[q]
        self._deps(e, reads, writes)
        if key not in self.dsem:
            self.dsem[key] = Sem(self.nc.alloc_semaphore("d_" + str(key)))
        rec = self.dsem[key]
        ins = e.h.dma_start(out=out, in_=in_)
        self.ninst += 1
        rec.count += 16
        ins.then_inc(rec.handle, 16)
        self._mark((rec, rec.count), reads, writes)
        return ins

    def barrier(self):
        toks = [(e.sem, e.sem.count) for e in self.eng.values() if e.sem is not None]
        toks += [(r, r.count) for r in self.dsem.values()]
        for e in self.eng.values():
            for s, v in toks:
                if v == 0 or e.seen.get(s, 0) >= v:
                    continue
                e.h.wait_ge(s.handle, v)
                e.seen[s] = v
                self.ninst += 1
        for c in self.cells:
            c.w = None
            c.r = {}


class Rot:
    def __init__(self, items):
        self.items = list(items)
        self.i = 0

    def next(self):
        it = self.items[self.i % len(self.items)]
        self.i += 1
        return it


def _rope_tables(S):
    pos = np.arange(S, dtype=np.float32)
    inv_freq = (np.float32(ROPE_THETA) ** (-np.arange(0, 16, 2, dtype=np.float32) / np.float32(16))).astype(np.float32)
    ang = (pos[:, None] * inv_freq[None, :]).astype(np.float32)
    return np.cos(ang).astype(np.float32), np.sin(ang).astype(np.float32)


def _pool_blocks(S):
    NT = S // 128
    out = np.zeros((128, NG, 5, 128), dtype=np.float32)
    pos = np.arange(S)
    for g, w in enumerate(POOL_WINDOWS):
        left = w // 2
        right = w - 1 - left
        lo = np.clip(pos - left, 0, S)
        hi = np.clip(pos + right + 1, 0, S)
        cnt = (hi - lo).astype(np.float32)

        def blk(i, rel):
            b = np.zeros((128, 128), dtype=np.float32)
            for t in range(128):
                tg = i * 128 + t
                for tpg in range(lo[tg], hi[tg]):
                    tp = tpg - (i + rel) * 128
                    if 0 <= tp < 128:
                        b[tp, t] += 1.0 / cnt[tg]
                if rel == 0:
                    b[t, t] -= 1.0
            return b

        mid = min(1, NT - 1)
        out[:, g, 0, :] = blk(mid, -1) if NT > 1 else 0
        out[:, g, 1, :] = blk(mid, 0)
        out[:, g, 2, :] = blk(mid, 1) if NT > 2 else (blk(0, 1) if NT > 1 else 0)
        out[:, g, 3, :] = blk(0, 0)
        out[:, g, 4, :] = blk(NT - 1, 0)
    return out


def build_nc(S, NSEQ, n_exp=NE, dbg=False, stop_after=None):
    NT = S // 128
    NQC = S // 512
    nc = bass.Bass("TRN2", target_bir_lowering=False)
    P = Prog(nc)

    def din(name, shape, dt=F32):
        return nc.dram_tensor(name, list(shape), dt, kind="ExternalInput").ap()

    x_d = din("x", [NSEQ, S, D])
    ct_d = din("c_t", [128, KT, NSEQ])
    wada_d = din("w_ada", [D, N_MOD * D])
    badaT_d = din("b_adaT", [128, N_MOD * KT])
    gT_d = din("gT", [128, 3, KT])
    win_d = din("w_in", [D, 2048])
    lam_d = din("lam4", [4, 64])
    subln_d = din("subln", [128, 1])
    wpool_d = din("w_pool", [NG, 128, 128])
    pscale_d = din("pscaleT", [128, NG])
    wout_d = din("w_out", [D, D])
    wr_d = din("w_r", [D, 36])
    br_d = din("b_r", [1, 36])
    wg_d = din("w_gate", [NE, D, DE])
    wu_d = din("w_up", [NE, D, DE])
    wd_d = din("w_down", [NE, DE, D])
    ident_d = din("ident", [128, 128])
    cos_d = din("cosT", [128, NT, 8])
    sin_d = din("sinT", [128, NT, 8])
    ab_d = din("Ab", [128, NG, 5, 128])
    out_d = nc.dram_tensor("out", [NSEQ, S, D], F32, kind="ExternalOutput").ap()
    dbg_d = {}

    def sb(name, shape, dt=F32):
        return nc.alloc_sbuf_tensor(name, list(shape), dt)

    X_f32 = sb("regX", [128, NT * D], F32)
    X_bf = X_f32[:].bitcast(BF16)
    Y_bf = sb("regY", [128, KT * S], BF16)
    W_bf = sb("regW", [128, 2 * 12288], BF16)
    STG = [sb("stg%d" % i, [128, 2048], F32) for i in range(2)]
    T_f32 = sb("regT", [128, 4096], F32)
    BC = [sb("bc%d" % i, [128, D], F32) for i in range(2)]
    ident = sb("ident_sb", [128, 128], F32)
    ones32 = sb("ones32", [128, 128], F32)
    ones_bf = sb("ones_bf", [128, 128], BF16)
    cosT = sb("cos_sb", [128, NT, 8], F32)
    sinT = sb("sin_sb", [128, NT, 8], F32)
    Ab_bf = sb("ab_bf", [128, NG * 5 * 128], BF16)
    wpool_bf = sb("wpool_bf", [128, NG * 128], BF16)
    pscaleT = sb("pscale_sb", [128, NG], F32)
    small = sb("small", [128, 64], F32)
    lam_sb = sb("lam_sb", [128, 4 * 64], F32)
    wr_sb = sb("wr_sb", [128, KT * 36], F32)
    br_sb = sb("br_sb", [1, 36], F32)
    cS = sb("cS", [128, KT * NSEQ], F32)
    modT = sb("modT", [128, 48 * NSEQ], F32)
    badaT = sb("badaT", [128, 48], F32)
    gT = sb("gT_sb", [128, 3 * KT], F32)
    cvec = sb("cvec", [128, 3 * KT], F32)
    rep = sb("rep", [128, KT * 128], F32)
    ssq = sb("ssq", [128, 2 * NT], F32)
    rstd = sb("rstd", [128, 2 * NT], F32)
    Wdn = sb("Wdense", [128, NT * NE], F32)
    rt = sb("rt", [128, 256], F32)
    GLb = sb("GLb", [128, NT * NG], F32)
    ELb = sb("ELb", [128, NT * NE], F32)
    junk = sb("junk", [128, D], BF16)

    banks = [nc.alloc_psum_tensor("bank%d" % i, [128, 512], F32) for i in range(8)]
    bcell = [P.cell("bank%d" % i, excl=True) for i in range(8)]

    c_const = P.cell("const")
    c_stg = [P.cell("stg0"), P.cell("stg1")]
    c_X = [P.cell("X%d" % i) for i in range(NT)]
    c_Y = [P.cell("Y%d" % i) for i in range(KT)]
    c_Yt = [P.cell("Yt%d" % i) for i in range(NT)]
    c_W = [P.cell("W%d" % i) for i in range(6)]
    c_T = [P.cell("T%d" % i) for i in range(8)]
    c_BC = [P.cell("bc0"), P.cell("bc1")]
    c_small = P.cell("small")
    c_cvec = P.cell("cvec")
    c_rep = P.cell("rep")
    c_ssq = P.cell("ssq")
    c_rstd = P.cell("rstd")
    c_Wdn = P.cell("Wdn")
    c_rt = P.cell("rt")
    c_lg = P.cell("lg")
    c_junk = P.cell("junk")
    c_modT = P.cell("modT")
    c_cS = P.cell("cS")

    def bank_ap(b):
        return banks[b][:]

    def xs_tile(i):
        return X_f32[:, i * D:(i + 1) * D]

    def qT_v():
        return X_bf[:, 0:4 * S].rearrange("p (h t) -> p h t", h=4)

    def kT_v():
        return X_bf[:, 4 * S:8 * S].rearrange("p (h t) -> p h t", h=4)

    def v_v():
        return X_bf[:, 8 * S:12 * S].rearrange("p (i n) -> p i n", n=512)

    def u_v():
        return X_bf[:, 12 * S:16 * S].rearrange("p (i n) -> p i n", n=512)

    def xcells(lo_bf, hi_bf):
        a = (lo_bf * 2) // (4 * D)
        b = (hi_bf * 2 - 1) // (4 * D)
        return [c_X[k] for k in range(a, b + 1)]

    cQ = xcells(0, 4 * S)
    cK = xcells(4 * S, 8 * S)
    cV = xcells(8 * S, 12 * S)
    cU = xcells(12 * S, 16 * S)

    YT = Y_bf[:].rearrange("p (k t) -> p k t", k=KT)

    def load_const(dst_ap, src_ap):
        P.dma("sp", dst_ap, src_ap, writes=[c_const], key="const")

    def mm(out, lhsT, rhs, start, stop, reads, writes):
        P.op("pe", lambda h: h.matmul(out, lhsT=lhsT, rhs=rhs, start=start, stop=stop),
             reads=reads, writes=writes, inc=stop)

    def tr(out, in_, reads, writes, inc=True):
        P.op("pe", lambda h: h.transpose(out=out, in_=in_, identity=ident[:]),
             reads=list(reads) + [c_const], writes=writes, inc=inc)

    def bcast(src_compact, dst_i, bks):
        P.op("dve", lambda h: h.tensor_copy(
            out=rep[:].rearrange("p (k m) -> p k m", k=KT),
            in_=src_compact.unsqueeze(2).to_broadcast([128, KT, 128])),
            reads=[c_cvec, c_modT, c_const], writes=[c_rep])
        for half in range(2):
            b = bks[half]
            for j in range(4):
                kt = half * 4 + j
                mm(banks[b][:, j * 128:(j + 1) * 128], rep[:, kt * 128:(kt + 1) * 128], ident[:],
                   True, True, [c_rep, c_const], [bcell[b]])
            P.op("act", lambda h, b=b, half=half: h.copy(out=BC[dst_i][:, half * 512:(half + 1) * 512], in_=bank_ap(b)),
                 reads=[bcell[b]], writes=[c_BC[dst_i]])

    def rstd_from_ssq(col, n, inv_n):
        P.op("act", lambda h: h.activation(out=rstd[:, col:col + n], in_=ssq[:, col:col + n], func=AF.Ln,
                                           scale=inv_n, bias=small[:, 2:3]),
             reads=[c_ssq, c_small], writes=[c_rstd])
        P.op("act", lambda h: h.activation(out=rstd[:, col:col + n], in_=rstd[:, col:col + n], func=AF.Exp, scale=-0.5),
             reads=[c_rstd], writes=[c_rstd])

    class _Stop(Exception):
        pass

    def maybe_stop(tag):
        if stop_after == tag:
            P.barrier()
            raise _Stop()

    try:
        load_const(ident[:], ident_d[:, :])
        load_const(cosT[:], cos_d[:, :, :])
        load_const(sinT[:], sin_d[:, :, :])
        load_const(pscaleT[:], pscale_d[:, :])
        load_const(small[:, 0:1], subln_d[:, :])
        load_const(lam_sb[:], lam_d.rearrange("a b -> (a b)").partition_broadcast(128))
        load_const(wr_sb[:].rearrange("p (k n) -> p k n", k=KT), wr_d.rearrange("(k p) n -> p k n", p=128))
        load_const(br_sb[:], br_d[:, :])
        load_const(cS[:].rearrange("p (k s) -> p k s", k=KT), ct_d[:, :, :])
        load_const(badaT[:], badaT_d[:, :])
        load_const(gT[:].rearrange("p (a k) -> p a k", a=3), gT_d[:, :, :])
        for hf in range(2):
            P.dma("sp", STG[hf][:, 0:2 * 5 * 128].rearrange("p (g v t) -> p g v t", g=2, v=5),
                  ab_d[:, hf * 2:(hf + 1) * 2, :, :], writes=[c_stg[hf]], key="stg%d" % hf)
            P.op("dve", lambda h: h.tensor_copy(out=Ab_bf[:, hf * 1280:(hf + 1) * 1280], in_=STG[hf][:, 0:1280]),
                 reads=[c_stg[hf]], writes=[c_const])
        P.dma("sp", STG[0][:, 0:NG * 128].rearrange("p (g d) -> p g d", g=NG), wpool_d.rearrange("g c d -> c g d"),
              writes=[c_stg[0]], key="stg0")
        P.op("dve", lambda h: h.tensor_copy(out=wpool_bf[:], in_=STG[0][:, 0:NG * 128]), reads=[c_stg[0]], writes=[c_const])
        P.op("dve", lambda h: h.memset(ones32[:], 1.0), writes=[c_const])
        P.op("dve", lambda h: h.memset(ones_bf[:], 1.0), writes=[c_const])
        P.op("dve", lambda h: h.memset(small[:, 2:3], RMS_EPS), writes=[c_small])
        P.barrier()
        maybe_stop('const')
        P.op("dve", lambda h: h.tensor_scalar(out=small[:, 1:2], in0=small[:, 0:1], scalar1=float(1.0 - LAMBDA_INIT),
                                              scalar2=None, op0=ALU.mult), reads=[c_small], writes=[c_small])
        lam3 = lam_sb[:].rearrange("p (a b) -> p a b", a=4)
        P.op("dve", lambda h: h.tensor_tensor(out=rt[:, 0:64], in0=lam3[:, 0, :], in1=lam3[:, 1, :], op=ALU.mult),
             reads=[c_const], writes=[c_rt])
        P.op("dve", lambda h: h.tensor_tensor(out=rt[:, 64:128], in0=lam3[:, 2, :], in1=lam3[:, 3, :], op=ALU.mult),
             reads=[c_const], writes=[c_rt])
        P.op("dve", lambda h: h.tensor_reduce(out=small[:, 4:6], in_=rt[:, 0:128].rearrange("p (a b) -> p a b", a=2),
                                              axis=AX.X, op=ALU.add), reads=[c_rt], writes=[c_small])
        P.op("act", lambda h: h.activation(out=small[:, 6:8], in_=small[:, 4:6], func=AF.Exp), reads=[c_small], writes=[c_small])
        P.op("dve", lambda h: h.tensor_tensor(out=small[:, 3:4], in0=small[:, 7:8], in1=small[:, 6:7], op=ALU.subtract),
             reads=[c_small], writes=[c_small])
        P.op("dve", lambda h: h.tensor_scalar(out=small[:, 3:4], in0=small[:, 3:4], scalar1=float(-LAMBDA_INIT),
                                              scalar2=None, op0=ALU.add), reads=[c_small], writes=[c_small])
        P.op("act", lambda h: h.activation(out=cS[:], in_=cS[:], func=AF.Silu), reads=[c_const], writes=[c_cS])

        cS3 = cS[:].rearrange("p (k s) -> p k s", k=KT)
        for j in range(12):
            for hf in range(2):
                P.dma("sp", STG[hf][:].rearrange("p (k n) -> p k n", k=4),
                      wada_d[hf * 512:(hf + 1) * 512, j * 512:(j + 1) * 512].rearrange("(k p) n -> p k n", p=128),
                      writes=[c_stg[hf]], key="stg%d" % hf)
            b = 1 + (j % 2)
            for kt in range(KT):
                st = kt // 4
                mm(banks[b][0:NSEQ, :], cS3[:, kt, :], STG[st][:, (kt % 4) * 512:(kt % 4 + 1) * 512],
                   kt == 0, kt == KT - 1, [c_stg[st], c_cS], [bcell[b]])
            rowb = rep[0:NSEQ, (j % 2) * 512:(j % 2 + 1) * 512]
            P.op("act", lambda h: h.copy(out=rowb, in_=banks[b][0:NSEQ, :]), reads=[bcell[b]], writes=[c_rep])
            for nt in range(4):
                col = (j * 4 + nt) * NSEQ
                P.op("pe", lambda h: h.transpose(out=banks[0][:, col:col + NSEQ], in_=rowb[:, nt * 128:(nt + 1) * 128],
                                                 identity=ident[0:NSEQ, 0:NSEQ]),
                     reads=[c_rep, c_const], writes=[bcell[0]])
        P.op("dve", lambda h: h.tensor_tensor(
            out=modT[:].rearrange("p (n s) -> p n s", s=NSEQ),
            in0=banks[0][:, 0:48 * NSEQ].rearrange("p (n s) -> p n s", s=NSEQ),
            in1=badaT[:].unsqueeze(2).to_broadcast([128, 48, NSEQ]), op=ALU.add),
            reads=[bcell[0], c_const], writes=[c_modT])
        P.barrier()
        maybe_stop('P0')

        modT3 = modT[:].rearrange("p (n s) -> p n s", s=NSEQ)
        gT3 = gT[:].rearrange("p (a k) -> p a k", a=3)
        cv3 = cvec[:].rearrange("p (a k) -> p a k", a=3)

        def mod_vec(j, s):
            return modT3[:, j * KT:(j + 1) * KT, s]

        def norm_mod_transpose(s, a_idx, sh_idx, g_idx, src_tile, src_cells, dst_hook):
            P.op("dve", lambda h: h.scalar_tensor_tensor(out=cv3[:, 0, :], in0=mod_vec(a_idx, s), scalar=1.0,
                                                         in1=gT3[:, g_idx, :], op0=ALU.add, op1=ALU.mult),
                 reads=[c_modT, c_const], writes=[c_cvec])
            bcast(cv3[:, 0, :], 0, (0, 1))
            bcast(mod_vec(sh_idx, s), 1, (2, 3))
            pairs = Rot([(0, 1), (2, 3)])
            hN = [T_f32[:, 0:D], T_f32[:, D:2 * D]]
            hNc = [[c_T[0], c_T[1]], [c_T[2], c_T[3]]]
            for i in range(NT):
                xt, xc = src_tile(i)
                P.op("dve", lambda h: h.scalar_tensor_tensor(out=junk[:], in0=xt, scalar=1.0, in1=xt, op0=ALU.mult,
                                                             op1=ALU.mult, accum_out=ssq[:, i:i + 1]),
                     reads=xc, writes=[c_junk, c_ssq])
                rstd_from_ssq(i, 1, 1.0 / D)
                hb = i % 2
                P.op("dve", lambda h: h.scalar_tensor_tensor(out=hN[hb], in0=xt, scalar=rstd[:, i:i + 1], in1=BC[0][:],
                                                             op0=ALU.mult, op1=ALU.mult),
                     reads=list(xc) + [c_rstd, c_BC[0]], writes=hNc[hb])
                P.op("dve", lambda h: h.tensor_tensor(out=hN[hb], in0=hN[hb], in1=BC[1][:], op=ALU.add),
                     reads=hNc[hb] + [c_BC[1]], writes=hNc[hb])
                ba, bb = pairs.next()
                for kt in range(KT):
                    b = ba if kt < 4 else bb
                    tr(banks[b][:, (kt % 4) * 128:(kt % 4 + 1) * 128], hN[hb][:, kt * 128:(kt + 1) * 128],
                       hNc[hb], [bcell[b]])
                dst_hook(i, ba, bb)

        def store_dbg(name, ap_sb, shape, dt, cells):
            t = nc.dram_tensor("dbg_" + name, list(shape), dt, kind="ExternalOutput").ap()
            dbg_d[name] = t
            P.dma("sp", t, ap_sb, reads=cells, key="dbg")

        for s in range(NSEQ):
            xl = [T_f32[:, 2 * D:3 * D], T_f32[:, 3 * D:4 * D]]
            xlc = [[c_T[4], c_T[5]], [c_T[6], c_T[7]]]

            def src_x(i):
                P.dma("sp", xl[i % 2], x_d[s, i * 128:(i + 1) * 128, :], writes=xlc[i % 2], key="xl%d" % (i % 2))
                return xl[i % 2], xlc[i % 2]

            def h1_hook(i, ba, bb):
                for half, b in enumerate((ba, bb)):
                    P.op("act", lambda h, b=b, half=half: h.copy(
                        out=YT[:, half * 4:(half + 1) * 4, i * 128:(i + 1) * 128],
                        in_=banks[b][:].rearrange("p (k t) -> p k t", k=4)),
                        reads=[bcell[b]], writes=[c_Y[half * 4 + k] for k in range(4)])

            norm_mod_transpose(s, 1, 0, 0, src_x, None, h1_hook)
            if dbg and s == 0:
                store_dbg("h1T", Y_bf[:], [128, KT * S], BF16, c_Y)
            maybe_stop('A1n')

            wbf = [W_bf[:, 0:4096].rearrange("p (k n) -> p k n", k=KT), W_bf[:, 4096:8192].rearrange("p (k n) -> p k n", k=KT)]
            wbc = [c_W[0], c_W[1]]
            prj = Rot([4, 5, 6, 7])
            trb = Rot([0, 1, 2, 3])
            qtm = [T_f32[:, 2 * D:2 * D + 512], T_f32[:, 2 * D + 512:3 * D]]
            qtmc = [c_T[4], c_T[5]]
            tmpr = T_f32[:, 3 * D:3 * D + 256]
            pend_tr = []
            for cch in range(4):
                wb = cch % 2
                for hf in range(2):
                    P.dma("sp", STG[hf][:].rearrange("p (k n) -> p k n", k=4),
                          win_d[hf * 512:(hf + 1) * 512, cch * 512:(cch + 1) * 512].rearrange("(k p) n -> p k n", p=128),
                          writes=[c_stg[hf]], key="stg%d" % hf)
                    P.op("pool", lambda h, hf=hf: h.tensor_copy(out=wbf[wb][:, hf * 4:(hf + 1) * 4, :],
                                                                in_=STG[hf][:].rearrange("p (k n) -> p k n", k=4)),
                         reads=[c_stg[hf]], writes=[wbc[wb]])
                for i in range(NT):
                    b = prj.next()
                    for kt in range(KT):
                        mm(banks[b][:], YT[:, kt, i * 128:(i + 1) * 128], wbf[wb][:, kt, :], kt == 0, kt == KT - 1,
                           [c_Y[kt], wbc[wb]], [bcell[b]])
                    while pend_tr:
                        pend_tr.pop(0)()
                    if cch < 2:
                        qb = i % 2
                        z3 = banks[b][:].rearrange("p (a d) -> p a d", a=8)
                        q3 = qtm[qb].rearrange("p (a d) -> p a d", a=8)
                        cosb = cosT[:, i, :].unsqueeze(1).to_broadcast([128, 8, 8])
                        sinb = sinT[:, i, :].unsqueeze(1).to_broadcast([128, 8, 8])
                        tA = tmpr[:, 0:64].rearrange("p (a d) -> p a d", a=8)
                        tB = tmpr[:, 64:128].rearrange("p (a d) -> p a d", a=8)
                        P.op("act", lambda h: h.copy(out=qtm[qb], in_=banks[b][:]), reads=[bcell[b]], writes=[qtmc[qb]])
                        P.op("dve", lambda h: h.tensor_tensor(out=tA, in0=z3[:, :, 0:8], in1=cosb, op=ALU.mult),
                             reads=[bcell[b], c_const], writes=[c_T[6]])
                        P.op("dve", lambda h: h.tensor_tensor(out=tB, in0=z3[:, :, 8:16], in1=sinb, op=ALU.mult),
                             reads=[bcell[b], c_const], writes=[c_T[6]])
                        P.op("dve", lambda h: h.tensor_tensor(out=q3[:, :, 0:8], in0=tA, in1=tB, op=ALU.subtract),
                             reads=[c_T[6]], writes=[qtmc[qb]])
                        P.op("dve", lambda h: h.tensor_tensor(out=tA, in0=z3[:, :, 8:16], in1=cosb, op=ALU.mult),
                             reads=[bcell[b], c_const], writes=[c_T[6]])
                        P.op("dve", lambda h: h.tensor_tensor(out=tB, in0=z3[:, :, 0:8], in1=sinb, op=ALU.mult),
                             reads=[bcell[b], c_const], writes=[c_T[6]])
                        P.op("dve", lambda h: h.tensor_tensor(out=q3[:, :, 8:16], in0=tA, in1=tB, op=ALU.add),
                             reads=[c_T[6]], writes=[qtmc[qb]])
                        def _tr_evac(qb=qb, i=i, cch=cch):
                            tb_ = trb.next()
                            for hh in range(NH):
                                tr(banks[tb_][:, hh * 128:(hh + 1) * 128], qtm[qb][:, hh * 128:(hh + 1) * 128],
                                   [qtmc[qb]], [bcell[tb_]])
                            dstv = qT_v() if cch == 0 else kT_v()
                            P.op("act", lambda h: h.copy(out=dstv[:, :, i * 128:(i + 1) * 128],
                                                         in_=banks[tb_][:].rearrange("p (k t) -> p k t", k=4)),
                                 reads=[bcell[tb_]], writes=(cQ if cch == 0 else cK))
                        pend_tr.append(_tr_evac)
                    else:
                        dstv = v_v() if cch == 2 else u_v()
                        P.op("act", lambda h: h.copy(out=dstv[:, i, :], in_=banks[b][:]),
                             reads=[bcell[b]], writes=(cV if cch == 2 else cU))
            while pend_tr:
                pend_tr.pop(0)()
            P.barrier()
            if dbg and s == 0:
                store_dbg("qkvu", X_bf[:], [128, 2 * NT * D], BF16, c_X)
                P.barrier()

            maybe_stop('A1')
            mixT = YT
            qT, kT, vv, uu = qT_v(), kT_v(), v_v(), u_v()
            srot = Rot([0, 1, 2, 3])
            Pb = [W_bf[:, 8192 + k * 512:8192 + (k + 1) * 512] for k in range(4)]
            Pc = [P.cell("P%d" % k) for k in range(4)]
            prot = Rot([0, 1, 2, 3])
            tA_, tB_, tR_, tO_ = (T_f32[:, k * 512:(k + 1) * 512] for k in range(4))
            cA, cB, cR, cO = c_T[0], c_T[1], c_T[2], c_T[3]
            tSq = T_f32[:, 2048:2560]
            cSq = c_T[4]
            steps = [(hh, qc, kt, comp) for hh in range(NH) for qc in range(NQC) for kt in range(NT) for comp in range(2)]
            LA = 2
            pk_of = {}
            deferred = []

            def emit_S(idx):
                hh, qc, kt, comp = steps[idx]
                qs = slice(qc * 512, (qc + 1) * 512)
                ks = slice(kt * 128, (kt + 1) * 128)
                ps = slice(comp * 64, (comp + 1) * 64)
                sbk = srot.next()
                mm(banks[sbk][:], kT[ps, hh, ks], qT[ps, hh, qs], True, True, cQ + cK, [bcell[sbk]])
                pk = prot.next()
                pk_of[idx] = pk
                P.op("act", lambda h: h.activation(out=Pb[pk], in_=banks[sbk][:], func=AF.Exp, scale=0.125),
                     reads=[bcell[sbk]], writes=[Pc[pk]])

            sL1 = T_f32[:, 2560:3072]
            sL2 = T_f32[:, 3072:3584]
            cL1, cL2 = c_T[5], c_T[6]

            def post1(hh, qc):
                P.op("act", lambda h: h.copy(out=tA_, in_=banks[4][:]), reads=[bcell[4]], writes=[cA])
                P.op("dve", lambda h: h.tensor_copy(out=sL1, in_=banks[5][:]), reads=[bcell[5]], writes=[cL1])
                P.op("act", lambda h: h.copy(out=tB_, in_=banks[6][:]), reads=[bcell[6]], writes=[cB])
                P.op("dve", lambda h: h.tensor_copy(out=sL2, in_=banks[7][:]), reads=[bcell[7]], writes=[cL2])
                P.op("dve", lambda h: h.reciprocal(out=sL1, in_=sL1), reads=[cL1], writes=[cL1])
                P.op("dve", lambda h: h.tensor_tensor(out=tA_, in0=tA_, in1=sL1, op=ALU.mult), reads=[cA, cL1], writes=[cA])
                P.op("dve", lambda h: h.reciprocal(out=sL2, in_=sL2), reads=[cL2], writes=[cL2])
                P.op("dve", lambda h: h.tensor_tensor(out=tB_, in0=tB_, in1=sL2, op=ALU.mult), reads=[cB, cL2], writes=[cB])
                P.op("dve", lambda h: h.scalar_tensor_tensor(out=tO_, in0=tB_, scalar=small[:, 3:4], in1=tA_,
                                                             op0=ALU.mult, op1=ALU.add),
                     reads=[cA, cB, c_small], writes=[cO])
                P.op("dve", lambda h: h.tensor_tensor(out=tSq, in0=tO_, in1=tO_, op=ALU.mult), reads=[cO], writes=[cSq])

            def post2(hh, qc):
                qs = slice(qc * 512, (qc + 1) * 512)
                sb2 = srot.next()
                mm(banks[sb2][:], ones32[:], tSq, True, True, [c_const, cSq], [bcell[sb2]])
                P.op("act", lambda h: h.activation(out=tR_, in_=banks[sb2][:], func=AF.Ln, scale=1.0 / 128, bias=small[:, 2:3]),
                     reads=[bcell[sb2], c_small], writes=[cR])
                P.op("act", lambda h: h.activation(out=tR_, in_=tR_, func=AF.Exp, scale=-0.5), reads=[cR], writes=[cR])
                P.op("dve", lambda h: h.scalar_tensor_tensor(out=mixT[:, hh, qs], in0=tO_, scalar=small[:, 1:2], in1=tR_,
                                                             op0=ALU.mult, op1=ALU.mult),
                     reads=[cO, cR, c_small], writes=[c_Y[hh]])

            def emit_AV(idx):
                hh, qc, kt, comp = steps[idx]
                pk = pk_of.pop(idx)
                ob, lb = (4, 5) if comp == 0 else (6, 7)
                mm(banks[ob][:], vv[:, kt, hh * 128:(hh + 1) * 128], Pb[pk], kt == 0, kt == NT - 1,
                   cV + [Pc[pk]], [bcell[ob]])
                mm(banks[lb][:], ones_bf[:], Pb[pk], kt == 0, kt == NT - 1, [c_const, Pc[pk]], [bcell[lb]])
                if kt == NT - 1 and comp == 1:
                    post1(hh, qc)
                    deferred.append((idx + min(14, 2 * NT - 2), (hh, qc)))

            npairs = len(steps) // 2
            LAp = 1
            for p_ in range(npairs + LAp):
                if p_ < npairs:
                    emit_S(2 * p_)
                    emit_S(2 * p_ + 1)
                j = p_ - LAp
                if j >= 0:
                    emit_AV(2 * j)
                    emit_AV(2 * j + 1)
                    while deferred and deferred[0][0] <= 2 * j + 1:
                        _, (h_, q_) = deferred.pop(0)
                        post2(h_, q_)
            while deferred:
                _, (h_, q_) = deferred.pop(0)
                post2(h_, q_)
            maybe_stop('A2')
            Ab4 = Ab_bf[:].rearrange("p (g v t) -> p g v t", g=NG, v=5)
            wp3 = wpool_bf[:].rearrange("p (g d) -> p g d", g=NG)
            dT = [W_bf[:, 8192 + 2048 + k * 512:8192 + 2048 + (k + 1) * 512] for k in range(2)]
            dTc = [P.cell("dT0"), P.cell("dT1")]
            for g in range(NG):
                for j in range(NQC):
                    b = srot.next()
                    for tt in range(4):
                        i = j * 4 + tt
                        rels = [r for r in (-1, 0, 1) if 0 <= i + r < NT]
                        for n_, r in enumerate(rels):
                            if r == 0:
                                var = 3 if i == 0 else (4 if i == NT - 1 else 1)
                            else:
                                var = 0 if r == -1 else 2
                            mm(banks[b][:, tt * 128:(tt + 1) * 128], uu[:, i + r, g * 128:(g + 1) * 128], Ab4[:, g, var, :],
                               n_ == 0, n_ == len(rels) - 1, cU + [c_const], [bcell[b]])
                    db = (g * NQC + j) % 2
                    P.op("act", lambda h: h.copy(out=dT[db], in_=banks[b][:]), reads=[bcell[b]], writes=[dTc[db]])
                    b2 = srot.next()
                    mm(banks[b2][:], wp3[:, g, :], dT[db], True, True, [c_const, dTc[db]], [bcell[b2]])
                    P.op("dve", lambda h: h.tensor_scalar(out=mixT[:, 4 + g, j * 512:(j + 1) * 512], in0=banks[b2][:],
                                                          scalar1=pscaleT[:, g:g + 1], scalar2=None, op0=ALU.mult),
                         reads=[bcell[b2], c_const], writes=[c_Y[4 + g]])
            P.barrier()
            if dbg and s == 0:
                store_dbg("mixT", Y_bf[:], [128, KT * S], BF16, c_Y)
                P.barrier()

            maybe_stop('A2b')
            for i in range(NT):
                P.dma("sp", xs_tile(i), x_d[s, i * 128:(i + 1) * 128, :], writes=[c_X[i]], key="xs%d" % i)
            bcast(mod_vec(2, s), 0, (0, 1))
            wo = W_bf[:, 0:8192].rearrange("p (k n) -> p k n", k=KT)
            for cc in range(2):
                for hf in range(2):
                    P.dma("sp", STG[hf][:].rearrange("p (k n) -> p k n", k=4),
                          wout_d[hf * 512:(hf + 1) * 512, cc * 512:(cc + 1) * 512].rearrange("(k p) n -> p k n", p=128),
                          writes=[c_stg[hf]], key="stg%d" % hf)
                    P.op("pool", lambda h, hf=hf, cc=cc: h.tensor_tensor(
                        out=wo[:, hf * 4:(hf + 1) * 4, cc * 512:(cc + 1) * 512],
                        in0=STG[hf][:].rearrange("p (k n) -> p k n", k=4),
                        in1=BC[0][:, cc * 512:(cc + 1) * 512].unsqueeze(1).to_broadcast([128, 4, 512]), op=ALU.mult),
                        reads=[c_stg[hf], c_BC[0]], writes=[c_W[0], c_W[1]])
            orot = Rot([2, 3, 4, 5, 6, 7])
            for i in range(NT):
                for cc in range(2):
                    b = orot.next()
                    for kt in range(KT):
                        mm(banks[b][:], mixT[:, kt, i * 128:(i + 1) * 128], wo[:, kt, cc * 512:(cc + 1) * 512],
                           kt == 0, kt == KT - 1, [c_Y[kt], c_W[0], c_W[1]], [bcell[b]])
                    xsl = xs_tile(i)[:, cc * 512:(cc + 1) * 512]
                    P.op("dve", lambda h: h.tensor_tensor(out=xsl, in0=banks[b][:], in1=xsl, op=ALU.add),
                         reads=[bcell[b], c_X[i]], writes=[c_X[i]])
            P.barrier()
            if dbg and s == 0:
                store_dbg("x1", X_f32[:], [128, NT * D], F32, c_X)
                P.barrier()

            maybe_stop('A3')
            h2T = YT
            h32 = [T_f32[:, 2 * D:3 * D].rearrange("p (k t) -> p k t", k=KT), T_f32[:, 3 * D:4 * D].rearrange("p (k t) -> p k t", k=KT)]
            h32c = [[c_T[4], c_T[5]], [c_T[6], c_T[7]]]
            wr3 = wr_sb[:].rearrange("p (k n) -> p k n", k=KT)
            rrot = Rot([4, 5, 6, 7])
            Wd3 = Wdn[:].rearrange("p (i e) -> p i e", e=NE)

            def src_x1(i):
                return xs_tile(i), [c_X[i]]

            def h2_hook(i, ba, bb):
                hb = i % 2
                for half, b in enumerate((ba, bb)):
                    P.op("act", lambda h, b=b, half=half: h.copy(
                        out=h32[hb][:, half * 4:(half + 1) * 4, :], in_=banks[b][:].rearrange("p (k t) -> p k t", k=4)),
                        reads=[bcell[b]], writes=h32c[hb])
                P.op("pool", lambda h: h.tensor_copy(out=h2T[:, :, i * 128:(i + 1) * 128], in_=h32[hb]),
                     reads=h32c[hb], writes=c_Y)
                def _router(i=i, hb=hb):
                    rb = rrot.next()
                    for kt in range(KT):
                        mm(banks[rb][:, 0:36], h32[hb][:, kt, :], wr3[:, kt, :], kt == 0, False, h32c[hb] + [c_const], [bcell[rb]])
                    mm(banks[rb][:, 0:36], ones32[0:1, :], br_sb[0:1, :], False, True, [c_const], [bcell[rb]])

                    def _copies():
                        P.op("dve", lambda h: h.tensor_copy(out=GLb[:, i * NG:(i + 1) * NG], in_=banks[rb][:, 0:4]),
                             reads=[bcell[rb]], writes=[c_lg])
                        P.op("dve", lambda h: h.tensor_copy(out=ELb[:, i * NE:(i + 1) * NE], in_=banks[rb][:, 4:36]),
                             reads=[bcell[rb]], writes=[c_lg])
                    pend_cp.append(_copies)
                if len(pend_cp) >= 2:
                    pend_cp.pop(0)()
                while pend_rt:
                    pend_rt.pop(0)()
                pend_rt.append(_router)

            pend_rt = []
            pend_cp = []
            norm_mod_transpose(s, 4, 3, 1, src_x1, None, h2_hook)
            while pend_rt:
                pend_rt.pop(0)()
            while pend_cp:
                pend_cp.pop(0)()
            _o = [0]

            def TV(n):
                a_ = _o[0]
                _o[0] += n
                return T_f32[:, a_:a_ + n]

            RC = c_T
            GL3 = GLb[:].rearrange("p (i g) -> p i g", g=NG)
            EL3 = ELb[:].rearrange("p (a j) -> p a j", j=EPG)
            gmax = TV(NT); goh = TV(NT * NG); gsh = TV(NT * NG); gsum = TV(NT); gp = TV(NT)
            esel = TV(NT * NE); ein = TV(NT * EPG); m1 = TV(NT); mk1 = TV(NT * EPG); ein2 = TV(NT * EPG)
            m2 = TV(NT); mk2 = TV(NT * EPG); dm = TV(NT); ex = TV(NT); den = TV(NT); p1 = TV(NT); p2 = TV(NT)
            w1 = TV(NT); w2 = TV(NT); we = TV(NT * EPG); tm1 = TV(NT * EPG); tm2 = TV(NT * EPG)
            g3 = lambda ap: ap.rearrange("p (i g) -> p i g", g=NG)
            j3 = lambda ap: ap.rearrange("p (i j) -> p i j", j=EPG)
            bc = lambda ap, n: ap.unsqueeze(2).to_broadcast([128, NT, n])
            RD = [c_lg] + RC

            def dv(fn):
                P.op("dve", fn, reads=RD, writes=RC)

            dv(lambda h: h.tensor_reduce(out=gmax, in_=GL3, axis=AX.X, op=ALU.max))
            dv(lambda h: h.tensor_tensor(out=g3(goh), in0=GL3, in1=bc(gmax, NG), op=ALU.is_equal))
            dv(lambda h: h.tensor_tensor(out=g3(gsh), in0=GL3, in1=bc(gmax, NG), op=ALU.subtract))
            P.op("act", lambda h: h.activation(out=gsh, in_=gsh, func=AF.Exp), reads=RC, writes=RC)
            dv(lambda h: h.tensor_reduce(out=gsum, in_=g3(gsh), axis=AX.X, op=ALU.add))
            dv(lambda h: h.reciprocal(out=gp, in_=gsum))
            dv(lambda h: h.tensor_tensor(out=esel.rearrange("p (a j) -> p a j", j=EPG), in0=EL3,
                                         in1=goh.unsqueeze(2).to_broadcast([128, NT * NG, EPG]), op=ALU.mult))
            dv(lambda h: h.tensor_reduce(out=j3(ein), in_=esel.rearrange("p (i g j) -> p i j g", g=NG, j=EPG),
                                         axis=AX.X, op=ALU.add))
            dv(lambda h: h.tensor_reduce(out=m1, in_=j3(ein), axis=AX.X, op=ALU.max))
            dv(lambda h: h.tensor_tensor(out=j3(mk1), in0=j3(ein), in1=bc(m1, EPG), op=ALU.is_equal))
            dv(lambda h: h.scalar_tensor_tensor(out=ein2, in0=mk1, scalar=-1e30, in1=ein, op0=ALU.mult, op1=ALU.add))
            dv(lambda h: h.tensor_reduce(out=m2, in_=j3(ein2), axis=AX.X, op=ALU.max))
            dv(lambda h: h.tensor_tensor(out=j3(mk2), in0=j3(ein2), in1=bc(m2, EPG), op=ALU.is_equal))
            dv(lambda h: h.tensor_tensor(out=dm, in0=m2, in1=m1, op=ALU.subtract))
            P.op("act", lambda h: h.activation(out=ex, in_=dm, func=AF.Exp), reads=RC, writes=RC)
            dv(lambda h: h.tensor_scalar(out=den, in0=ex, scalar1=1.0, scalar2=None, op0=ALU.add))
            dv(lambda h: h.reciprocal(out=p1, in_=den))
            dv(lambda h: h.tensor_tensor(out=p2, in0=ex, in1=p1, op=ALU.mult))
            dv(lambda h: h.tensor_tensor(out=w1, in0=p1, in1=gp, op=ALU.mult))
            dv(lambda h: h.tensor_tensor(out=w2, in0=p2, in1=gp, op=ALU.mult))
            dv(lambda h: h.tensor_tensor(out=j3(tm1), in0=j3(mk1), in1=bc(w1, EPG), op=ALU.mult))
            dv(lambda h: h.tensor_tensor(out=j3(tm2), in0=j3(mk2), in1=bc(w2, EPG), op=ALU.mult))
            dv(lambda h: h.tensor_tensor(out=we, in0=tm1, in1=tm2, op=ALU.add))
            for g in range(NG):
                P.op("dve", lambda h: h.tensor_tensor(out=Wd3[:, :, g * EPG:(g + 1) * EPG], in0=j3(we),
                                                      in1=g3(goh)[:, :, g:g + 1].to_broadcast([128, NT, EPG]), op=ALU.mult),
                     reads=RC, writes=[c_Wdn])
            P.barrier()
            if dbg and s == 0:
                store_dbg("Wdn", Wdn[:], [128, NT * NE], F32, [c_Wdn])
                store_dbg("h2T", Y_bf[:], [128, KT * S], BF16, c_Y)
                P.barrier()

            maybe_stop('B1')
            bcast(mod_vec(5, s), 0, (0, 1))
            wslot = [(W_bf[:, sl * 12288:sl * 12288 + 4096].rearrange("p (k n) -> p k n", k=KT),
                      W_bf[:, sl * 12288 + 4096:sl * 12288 + 8192].rearrange("p (k n) -> p k n", k=KT),
                      W_bf[:, sl * 12288 + 8192:sl * 12288 + 12288].rearrange("p (f n) -> p f n", f=4)) for sl in range(2)]
            wcell = [[c_W[0], c_W[1], c_W[2]], [c_W[3], c_W[4], c_W[5]]]
            hid = [T_f32[:, 0:1024].bitcast(BF16).rearrange("p (f t) -> p f t", f=4),
                   T_f32[:, 1024:2048].bitcast(BF16).rearrange("p (f t) -> p f t", f=4)]
            hidc = [[c_T[0], c_T[1]], [c_T[2], c_T[3]]]
            sg = [T_f32[:, 2048:2560], T_f32[:, 2560:3072]]
            sgc = [c_T[4], c_T[5]]
            gurot = Rot([(0, 1), (2, 3)])
            yrot = Rot([4, 5, 6, 7])
            sgrot = Rot([0, 1])
            stg_i = [0]

            def stage_next():
                k = stg_i[0] % 2
                stg_i[0] += 1
                return k

            def load_expert(e):
                sl = e % 2
                wgv, wuv, wdv = wslot[sl]
                for which, (src, dstv) in enumerate(((wg_d, wgv), (wu_d, wuv))):
                    for hf in range(2):
                        st = stage_next()
                        P.dma("sp", STG[st][:].rearrange("p (k n) -> p k n", k=4),
                              src[e, hf * 512:(hf + 1) * 512, :].rearrange("(k p) n -> p k n", p=128),
                              writes=[c_stg[st]], key="stg%d" % st)
                        P.op("pool", lambda h, st=st, dstv=dstv, hf=hf: h.tensor_copy(
                            out=dstv[:, hf * 4:(hf + 1) * 4, :], in_=STG[st][:].rearrange("p (k n) -> p k n", k=4)),
                            reads=[c_stg[st]], writes=[wcell[sl][which]])
                for hf in range(2):
                    st = stage_next()
                    P.dma("sp", STG[st][:].rearrange("p (f n) -> p f n", f=2),
                          wd_d[e, hf * 256:(hf + 1) * 256, :].rearrange("(f p) n -> p f n", p=128),
                          writes=[c_stg[st]], key="stg%d" % st)
                    P.op("pool", lambda h, st=st, hf=hf: h.tensor_tensor(
                        out=wdv[:, hf * 2:(hf + 1) * 2, :], in0=STG[st][:].rearrange("p (f n) -> p f n", f=2),
                        in1=BC[0][:].unsqueeze(1).to_broadcast([128, 2, D]), op=ALU.mult),
                        reads=[c_stg[st], c_BC[0]], writes=[wcell[sl][2]])

            def gate_up(e, tc):
                sl = e % 2
                wgv, wuv, _ = wslot[sl]
                hb = tc % 2
                ts_ = slice(tc * 512, (tc + 1) * 512)
                for f in range(4):
                    gb, ub = gurot.next()
                    for kt in range(KT):
                        mm(banks[gb][:], wgv[:, kt, f * 128:(f + 1) * 128], h2T[:, kt, ts_], kt == 0, kt == KT - 1,
                           [wcell[sl][0], c_Y[kt]], [bcell[gb]])
                    for kt in range(KT):
                        mm(banks[ub][:], wuv[:, kt, f * 128:(f + 1) * 128], h2T[:, kt, ts_], kt == 0, kt == KT - 1,
                           [wcell[sl][1], c_Y[kt]], [bcell[ub]])
                    si = sgrot.next()
                    P.op("act", lambda h, gb=gb, si=si: h.activation(out=sg[si], in_=banks[gb][:], func=AF.Silu),
                         reads=[bcell[gb]], writes=[sgc[si]])
                    P.op("dve", lambda h, ub=ub, si=si, f=f: h.tensor_tensor(out=hid[hb][:, f, :], in0=banks[ub][:], in1=sg[si], op=ALU.mult),
                         reads=[bcell[ub], sgc[si]], writes=hidc[hb])

            def down(e, tc):
                sl = e % 2
                wdv = wslot[sl][2]
                hb = tc % 2
                for tt in range(4):
                    i = tc * 4 + tt
                    for cc in range(2):
                        yb = yrot.next()
                        for f in range(4):
                            mm(banks[yb][:], hid[hb][:, f, tt * 128:(tt + 1) * 128], wdv[:, f, cc * 512:(cc + 1) * 512],
                               f == 0, f == 3, hidc[hb] + [wcell[sl][2]], [bcell[yb]])
                        xsl = xs_tile(i)[:, cc * 512:(cc + 1) * 512]
                        P.op("dve", lambda h, yb=yb, xsl=xsl, i=i: h.scalar_tensor_tensor(
                            out=xsl, in0=banks[yb][:], scalar=Wd3[:, i, e:e + 1], in1=xsl, op0=ALU.mult, op1=ALU.add),
                            reads=[bcell[yb], c_Wdn, c_X[i]], writes=[c_X[i]])

            load_expert(0)
            for e in range(n_exp):
                if e + 1 < n_exp:
                    load_expert(e + 1)
                gate_up(e, 0)
                for tc in range(NQC):
                    if tc + 1 < NQC:
                        gate_up(e, tc + 1)
                    down(e, tc)
            P.barrier()

            maybe_stop('B2')
            bcast(gT3[:, 2, :], 1, (0, 1))
            for i in range(NT):
                xt = xs_tile(i)
                P.op("dve", lambda h: h.scalar_tensor_tensor(out=junk[:], in0=xt, scalar=1.0, in1=xt, op0=ALU.mult,
                                                             op1=ALU.mult, accum_out=ssq[:, NT + i:NT + i + 1]),
                     reads=[c_X[i]], writes=[c_junk, c_ssq])
                rstd_from_ssq(NT + i, 1, 1.0 / D)
                P.op("dve", lambda h: h.scalar_tensor_tensor(out=xt, in0=xt, scalar=rstd[:, NT + i:NT + i + 1], in1=BC[1][:],
                                                             op0=ALU.mult, op1=ALU.mult),
                     reads=[c_X[i], c_rstd, c_BC[1]], writes=[c_X[i]])
                P.dma("sp", out_d[s, i * 128:(i + 1) * 128, :], xt, reads=[c_X[i]], key="xs%d" % i)
            P.barrier()
    except _Stop:
        pass
    return nc, dbg_d, P


def _prep_shared(S, inputs):
    f = lambda a: np.ascontiguousarray(np.asarray(a, dtype=np.float32))
    cos, sin = _rope_tables(S)
    NT = S // 128
    sh = {
        "w_ada": f(inputs["w_ada"][0]),
        "b_adaT": f(np.asarray(inputs["b_ada"][0]).reshape(N_MOD * KT, 128).T),
        "gT": f(np.stack([np.asarray(inputs["norm1_g"][0]).reshape(KT, 128).T,
                          np.asarray(inputs["norm2_g"][0]).reshape(KT, 128).T,
                          np.asarray(inputs["final_g"]).reshape(KT, 128).T], axis=1)),
        "w_in": f(inputs["w_in"][0]),
        "lam4": f(np.stack([np.asarray(inputs["lambda_q1"][0]), np.asarray(inputs["lambda_k1"][0]),
                            np.asarray(inputs["lambda_q2"][0]), np.asarray(inputs["lambda_k2"][0])], axis=0)),
        "subln": f(np.asarray(inputs["subln_g"][0]).reshape(128, 1)),
        "w_pool": f(inputs["w_pool"][0]),
        "pscaleT": f(np.asarray(inputs["pool_scale"][0]).T),
        "w_out": f(inputs["w_out"][0]),
        "w_r": f(np.concatenate([np.asarray(inputs["w_router_group"][0]), np.asarray(inputs["w_router_expert"][0])], axis=1)),
        "b_r": f(np.concatenate([np.asarray(inputs["b_router_group"][0]), np.asarray(inputs["b_router_expert"][0])]).reshape(1, 36)),
        "w_gate": f(inputs["w_gate"][0]),
        "w_up": f(inputs["w_up"][0]),
        "w_down": f(inputs["w_down"][0]),
        "ident": np.eye(128, dtype=np.float32),
        "cosT": f(cos.reshape(NT, 128, 8).transpose(1, 0, 2)),
        "sinT": f(sin.reshape(NT, 128, 8).transpose(1, 0, 2)),
        "Ab": _pool_blocks(S),
    }
    return sh


_CACHE = {}


def run(inputs, dbg=False, n_exp=NE):
    x = np.asarray(inputs["x"], dtype=np.float32)
    c = np.asarray(inputs["c"], dtype=np.float32)
    B, S, _ = x.shape
    NSEQ = B // NCORES
    key = (S, NSEQ, n_exp, dbg)
    if key not in _CACHE:
        _CACHE[key] = build_nc(S, NSEQ, n_exp=n_exp, dbg=dbg)
    nc, dbg_d, P = _CACHE[key]
    sh = _prep_shared(S, inputs)
    in_maps = []
    for core in range(NCORES):
        m = dict(sh)
        m["x"] = np.ascontiguousarray(x[core * NSEQ:(core + 1) * NSEQ])
        cc = c[core * NSEQ:(core + 1) * NSEQ]
        m["c_t"] = np.ascontiguousarray(cc.reshape(NSEQ, KT, 128).transpose(2, 1, 0))
        in_maps.append(m)
    res = run_bass_kernel_spmd(nc, in_maps, core_ids=list(range(NCORES)))
    out = np.concatenate([r["out"] for r in res.results], axis=0)
    if dbg:
        return out, res.results
    return out


def kernel(**inputs):
    return run(inputs).astype(np.float32)
```

```python
import math
import numpy as np
import concourse.bass as bass
import concourse.mybir as mybir
from concourse.bass_utils import run_bass_kernel_spmd

F32 = mybir.dt.float32
BF16 = mybir.dt.bfloat16
AF = mybir.ActivationFunctionType
ALU = mybir.AluOpType
AX = mybir.AxisListType

D = 1024
KT = 8
NCORES = 8
N_MOD = 6
NH = 4
NG = 4
EPG = 8
NE = 32
DE = 512
RMS_EPS = 1e-6
LAMBDA_INIT = 0.8 - 0.6 * math.exp(-0.3 * 0)
ROPE_THETA = 500000.0
POOL_WINDOWS = (2, 4, 8, 16)


class Sem:
    def __init__(self, handle):
        self.handle = handle
        self.count = 0


class Cell:
    __slots__ = ("w", "r", "excl", "name")

    def __init__(self, name="", excl=False):
        self.w = None
        self.r = {}
        self.excl = excl
        self.name = name


class Eng:
    def __init__(self, name, h, sem):
        self.name = name
        self.h = h
        self.sem = sem
        self.seen = {}


class Prog:
    def __init__(self, nc):
        self.nc = nc
        self.cells = []
        self.eng = {}
        for name, h in (("pe", nc.tensor), ("act", nc.scalar), ("dve", nc.vector),
                        ("pool", nc.gpsimd), ("sp", nc.sync)):
            sem = Sem(nc.alloc_semaphore("s_" + name)) if name != "sp" else None
            self.eng[name] = Eng(name, h, sem)
        self.dsem = {}
        self.ninst = 0

    def cell(self, name="", excl=False):
        c = Cell(name, excl)
        self.cells.append(c)
        return c

    def _deps(self, e, reads, writes):
        deps = {}

        def need(s, v):
            if deps.get(s, 0) < v:
                deps[s] = v

        for c in reads:
            if c.w is not None:
                need(*c.w)
            if c.excl:
                for s, v in c.r.items():
                    need(s, v)
        for c in writes:
            if c.w is not None:
                need(*c.w)
            for s, v in c.r.items():
                need(s, v)
        for s, v in deps.items():
            if e.name == "pe" and s is e.sem:
                continue
            if e.seen.get(s, 0) >= v:
                continue
            e.h.wait_ge(s.handle, v)
            e.seen[s] = v
            self.ninst += 1

    def _mark(self, tok, reads, writes):
        s, v = tok
        for c in reads:
            if c.excl:
                c.w = tok
                c.r = {}
            else:
                if c.r.get(s, 0) < v:
                    c.r[s] = v
        for c in writes:
            c.w = tok
            c.r = {}

    def op(self, en, fn, reads=(), writes=(), inc=True):
        e = self.eng[en]
        self._deps(e, reads, writes)
        ins = fn(e.h)
        self.ninst += 1
        if inc:
            e.sem.count += 1
            ins.then_inc(e.sem.handle, 1)
            tok = (e.sem, e.sem.count)
        else:
            tok = (e.sem, e.sem.count + 1)
        self._mark(tok, reads, writes)
        return ins

    def dma(self, q, out, in_, reads=(), writes=(), key=None):
        e = self.eng[q]
        self._deps(e, reads, writes)
        if key not in self.dsem:
            self.dsem[key] = Sem(self.nc.alloc_semaphore("d_" + str(key)))
        rec = self.dsem[key]
        ins = e.h.dma_start(out=out, in_=in_)
        self.ninst += 1
        rec.count += 16
        ins.then_inc(rec.handle, 16)
        self._mark((rec, rec.count), reads, writes)
        return ins

    def barrier(self):
        toks = [(e.sem, e.sem.count) for e in self.eng.values() if e.sem is not None]
        toks += [(r, r.count) for r in self.dsem.values()]
        for e in self.eng.values():
            for s, v in toks:
                if v == 0 or e.seen.get(s, 0) >= v:
                    continue
                e.h.wait_ge(s.handle, v)
                e.seen[s] = v
                self.ninst += 1
        for c in self.cells:
            c.w = None
            c.r = {}


class Rot:
    def __init__(self, items):
        self.items = list(items)
        self.i = 0

    def next(self):
        it = self.items[self.i % len(self.items)]
        self.i += 1
        return it


def _rope_tables(S):
    pos = np.arange(S, dtype=np.float32)
    inv_freq = (np.float32(ROPE_THETA) ** (-np.arange(0, 16, 2, dtype=np.float32) / np.float32(16))).astype(np.float32)
    ang = (pos[:, None] * inv_freq[None, :]).astype(np.float32)
    return np.cos(ang).astype(np.float32), np.sin(ang).astype(np.float32)


def _pool_blocks(S):
    NT = S // 128
    out = np.zeros((128, NG, 5, 128), dtype=np.float32)
    pos = np.arange(S)
    for g, w in enumerate(POOL_WINDOWS):
        left = w // 2
        right = w - 1 - left
        lo = np.clip(pos - left, 0, S)
        hi = np.clip(pos + right + 1, 0, S)
        cnt = (hi - lo).astype(np.float32)

        def blk(i, rel):
            b = np.zeros((128, 128), dtype=np.float32)
            for t in range(128):
                tg = i * 128 + t
                for tpg in range(lo[tg], hi[tg]):
                    tp = tpg - (i + rel) * 128
                    if 0 <= tp < 128:
                        b[tp, t] += 1.0 / cnt[tg]
                if rel == 0:
                    b[t, t] -= 1.0
            return b

        mid = min(1, NT - 1)
        out[:, g, 0, :] = blk(mid, -1) if NT > 1 else 0
        out[:, g, 1, :] = blk(mid, 0)
        out[:, g, 2, :] = blk(mid, 1) if NT > 2 else (blk(0, 1) if NT > 1 else 0)
        out[:, g, 3, :] = blk(0, 0)
        out[:, g, 4, :] = blk(NT - 1, 0)
    return out


def build_nc(S, NSEQ, n_exp=NE, dbg=False, stop_after=None):
    NT = S // 128
    NQC = S // 512
    nc = bass.Bass("TRN2", target_bir_lowering=False)
    P = Prog(nc)

    def din(name, shape, dt=F32):
        return nc.dram_tensor(name, list(shape), dt, kind="ExternalInput").ap()

    x_d = din("x", [NSEQ, S, D])
    ct_d = din("c_t", [128, KT, NSEQ])
    wada_d = din("w_ada", [D, N_MOD * D])
    badaT_d = din("b_adaT", [128, N_MOD * KT])
    gT_d = din("gT", [128, 3, KT])
    win_d = din("w_in", [D, 2048])
    lam_d = din("lam4", [4, 64])
    subln_d = din("subln", [128, 1])
    wpool_d = din("w_pool", [NG, 128, 128])
    pscale_d = din("pscaleT", [128, NG])
    wout_d = din("w_out", [D, D])
    wr_d = din("w_r", [D, 36])
    br_d = din("b_r", [1, 36])
    wg_d = din("w_gate", [NE, D, DE])
    wu_d = din("w_up", [NE, D, DE])
    wd_d = din("w_down", [NE, DE, D])
    ident_d = din("ident", [128, 128])
    cos_d = din("cosT", [128, NT, 8])
    sin_d = din("sinT", [128, NT, 8])
    ab_d = din("Ab", [128, NG, 5, 128])
    out_d = nc.dram_tensor("out", [NSEQ, S, D], F32, kind="ExternalOutput").ap()
    dbg_d = {}

    def sb(name, shape, dt=F32):
        return nc.alloc_sbuf_tensor(name, list(shape), dt)

    X_f32 = sb("regX", [128, NT * D], F32)
    X_bf = X_f32[:].bitcast(BF16)
    Y_bf = sb("regY", [128, KT * S], BF16)
    W_bf = sb("regW", [128, 2 * 12288], BF16)
    STG = [sb("stg%d" % i, [128, 2048], F32) for i in range(2)]
    T_f32 = sb("regT", [128, 4096], F32)
    BC = [sb("bc%d" % i, [128, D], F32) for i in range(2)]
    ident = sb("ident_sb", [128, 128], F32)
    ones32 = sb("ones32", [128, 128], F32)
    ones_bf = sb("ones_bf", [128, 128], BF16)
    cosT = sb("cos_sb", [128, NT, 8], F32)
    sinT = sb("sin_sb", [128, NT, 8], F32)
    Ab_bf = sb("ab_bf", [128, NG * 5 * 128], BF16)
    wpool_bf = sb("wpool_bf", [128, NG * 128], BF16)
    pscaleT = sb("pscale_sb", [128, NG], F32)
    small = sb("small", [128, 64], F32)
    lam_sb = sb("lam_sb", [128, 4 * 64], F32)
    wr_sb = sb("wr_sb", [128, KT * 36], F32)
    br_sb = sb("br_sb", [1, 36], F32)
    cS = sb("cS", [128, KT * NSEQ], F32)
    modT = sb("modT", [128, 48 * NSEQ], F32)
    badaT = sb("badaT", [128, 48], F32)
    gT = sb("gT_sb", [128, 3 * KT], F32)
    cvec = sb("cvec", [128, 3 * KT], F32)
    rep = sb("rep", [128, KT * 128], F32)
    ssq = sb("ssq", [128, 2 * NT], F32)
    rstd = sb("rstd", [128, 2 * NT], F32)
    Wdn = sb("Wdense", [128, NT * NE], F32)
    rt = sb("rt", [128, 256], F32)
    GLb = sb("GLb", [128, NT * NG], F32)
    ELb = sb("ELb", [128, NT * NE], F32)
    junk = sb("junk", [128, D], BF16)

    bigb = [nc.alloc_psum_tensor("bigb%d" % i, [128, 1024], F32) for i in range(4)]
    banks = [bigb[i // 2][:, (i % 2) * 512:(i % 2 + 1) * 512] for i in range(8)]
    bcell = [P.cell("bank%d" % i, excl=True) for i in range(8)]

    c_const = P.cell("const")
    c_stg = [P.cell("stg0"), P.cell("stg1")]
    c_X = [P.cell("X%d" % i) for i in range(NT)]
    c_Y = [P.cell("Y%d" % i) for i in range(KT)]
    c_Yt = [P.cell("Yt%d" % i) for i in range(NT)]
    c_W = [P.cell("W%d" % i) for i in range(6)]
    c_T = [P.cell("T%d" % i) for i in range(8)]
    c_BC = [P.cell("bc0"), P.cell("bc1")]
    c_small = P.cell("small")
    c_cvec = P.cell("cvec")
    c_rep = P.cell("rep")
    c_ssq = P.cell("ssq")
    c_rstd = P.cell("rstd")
    c_Wdn = P.cell("Wdn")
    c_rt = P.cell("rt")
    c_lg = P.cell("lg")
    c_junk = P.cell("junk")
    c_modT = P.cell("modT")
    c_cS = P.cell("cS")

    def bank_ap(b):
        return banks[b][:]

    def xs_tile(i):
        return X_f32[:, i * D:(i + 1) * D]

    def qT_v():
        return X_bf[:, 0:4 * S].rearrange("p (h t) -> p h t", h=4)

    def kT_v():
        return X_bf[:, 4 * S:8 * S].rearrange("p (h t) -> p h t", h=4)

    def v_v():
        return X_bf[:, 8 * S:12 * S].rearrange("p (i n) -> p i n", n=512)

    def u_v():
        return X_bf[:, 12 * S:16 * S].rearrange("p (i n) -> p i n", n=512)

    def xcells(lo_bf, hi_bf):
        a = (lo_bf * 2) // (4 * D)
        b = (hi_bf * 2 - 1) // (4 * D)
        return [c_X[k] for k in range(a, b + 1)]

    cQ = xcells(0, 4 * S)
    cK = xcells(4 * S, 8 * S)
    cV = xcells(8 * S, 12 * S)
    cU = xcells(12 * S, 16 * S)

    YT = Y_bf[:].rearrange("p (k t) -> p k t", k=KT)

    def load_const(dst_ap, src_ap):
        P.dma("sp", dst_ap, src_ap, writes=[c_const], key="const")

    def mm(out, lhsT, rhs, start, stop, reads, writes):
        P.op("pe", lambda h: h.matmul(out, lhsT=lhsT, rhs=rhs, start=start, stop=stop),
             reads=reads, writes=writes, inc=stop)

    def tr(out, in_, reads, writes, inc=True):
        P.op("pe", lambda h: h.transpose(out=out, in_=in_, identity=ident[:]),
             reads=list(reads) + [c_const], writes=writes, inc=inc)

    def bcast(src_compact, dst_i, bks):
        P.op("dve", lambda h: h.tensor_copy(
            out=rep[:].rearrange("p (k m) -> p k m", k=KT),
            in_=src_compact.unsqueeze(2).to_broadcast([128, KT, 128])),
            reads=[c_cvec, c_modT, c_const], writes=[c_rep])
        for half in range(2):
            b = bks[half]
            for j in range(4):
                kt = half * 4 + j
                mm(banks[b][:, j * 128:(j + 1) * 128], rep[:, kt * 128:(kt + 1) * 128], ident[:],
                   True, True, [c_rep, c_const], [bcell[b]])
            P.op("act", lambda h, b=b, half=half: h.copy(out=BC[dst_i][:, half * 512:(half + 1) * 512], in_=bank_ap(b)),
                 reads=[bcell[b]], writes=[c_BC[dst_i]])

    def rstd_from_ssq(col, n, inv_n):
        P.op("act", lambda h: h.activation(out=rstd[:, col:col + n], in_=ssq[:, col:col + n], func=AF.Ln,
                                           scale=inv_n, bias=small[:, 2:3]),
             reads=[c_ssq, c_small], writes=[c_rstd])
        P.op("act", lambda h: h.activation(out=rstd[:, col:col + n], in_=rstd[:, col:col + n], func=AF.Exp, scale=-0.5),
             reads=[c_rstd], writes=[c_rstd])

    class _Stop(Exception):
        pass

    def maybe_stop(tag):
        if stop_after == tag:
            P.barrier()
            raise _Stop()

    try:
        load_const(ident[:], ident_d[:, :])
        load_const(cosT[:], cos_d[:, :, :])
        load_const(sinT[:], sin_d[:, :, :])
        load_const(pscaleT[:], pscale_d[:, :])
        load_const(small[:, 0:1], subln_d[:, :])
        load_const(lam_sb[:], lam_d.rearrange("a b -> (a b)").partition_broadcast(128))
        load_const(wr_sb[:].rearrange("p (k n) -> p k n", k=KT), wr_d.rearrange("(k p) n -> p k n", p=128))
        load_const(br_sb[:], br_d[:, :])
        load_const(cS[:].rearrange("p (k s) -> p k s", k=KT), ct_d[:, :, :])
        load_const(badaT[:], badaT_d[:, :])
        load_const(gT[:].rearrange("p (a k) -> p a k", a=3), gT_d[:, :, :])
        for hf in range(2):
            P.dma("sp", STG[hf][:, 0:2 * 5 * 128].rearrange("p (g v t) -> p g v t", g=2, v=5),
                  ab_d[:, hf * 2:(hf + 1) * 2, :, :], writes=[c_stg[hf]], key="stg%d" % hf)
            P.op("dve", lambda h: h.tensor_copy(out=Ab_bf[:, hf * 1280:(hf + 1) * 1280], in_=STG[hf][:, 0:1280]),
                 reads=[c_stg[hf]], writes=[c_const])
        P.dma("sp", STG[0][:, 0:NG * 128].rearrange("p (g d) -> p g d", g=NG), wpool_d.rearrange("g c d -> c g d"),
              writes=[c_stg[0]], key="stg0")
        P.op("dve", lambda h: h.tensor_copy(out=wpool_bf[:], in_=STG[0][:, 0:NG * 128]), reads=[c_stg[0]], writes=[c_const])
        P.op("dve", lambda h: h.memset(ones32[:], 1.0), writes=[c_const])
        P.op("dve", lambda h: h.memset(ones_bf[:], 1.0), writes=[c_const])
        P.op("dve", lambda h: h.memset(small[:, 2:3], RMS_EPS), writes=[c_small])
        P.barrier()
        maybe_stop('const')
        P.op("dve", lambda h: h.tensor_scalar(out=small[:, 1:2], in0=small[:, 0:1], scalar1=float(1.0 - LAMBDA_INIT),
                                              scalar2=None, op0=ALU.mult), reads=[c_small], writes=[c_small])
        lam3 = lam_sb[:].rearrange("p (a b) -> p a b", a=4)
        P.op("dve", lambda h: h.tensor_tensor(out=rt[:, 0:64], in0=lam3[:, 0, :], in1=lam3[:, 1, :], op=ALU.mult),
             reads=[c_const], writes=[c_rt])
        P.op("dve", lambda h: h.tensor_tensor(out=rt[:, 64:128], in0=lam3[:, 2, :], in1=lam3[:, 3, :], op=ALU.mult),
             reads=[c_const], writes=[c_rt])
        P.op("dve", lambda h: h.tensor_reduce(out=small[:, 4:6], in_=rt[:, 0:128].rearrange("p (a b) -> p a b", a=2),
                                              axis=AX.X, op=ALU.add), reads=[c_rt], writes=[c_small])
        P.op("act", lambda h: h.activation(out=small[:, 6:8], in_=small[:, 4:6], func=AF.Exp), reads=[c_small], writes=[c_small])
        P.op("dve", lambda h: h.tensor_tensor(out=small[:, 3:4], in0=small[:, 7:8], in1=small[:, 6:7], op=ALU.subtract),
             reads=[c_small], writes=[c_small])
        P.op("dve", lambda h: h.tensor_scalar(out=small[:, 3:4], in0=small[:, 3:4], scalar1=float(-LAMBDA_INIT),
                                              scalar2=None, op0=ALU.add), reads=[c_small], writes=[c_small])
        P.op("act", lambda h: h.activation(out=cS[:], in_=cS[:], func=AF.Silu), reads=[c_const], writes=[c_cS])

        cS3 = cS[:].rearrange("p (k s) -> p k s", k=KT)
        for j in range(12):
            for hf in range(2):
                P.dma("sp", STG[hf][:].rearrange("p (k n) -> p k n", k=4),
                      wada_d[hf * 512:(hf + 1) * 512, j * 512:(j + 1) * 512].rearrange("(k p) n -> p k n", p=128),
                      writes=[c_stg[hf]], key="stg%d" % hf)
            b = 1 + (j % 2)
            for kt in range(KT):
                st = kt // 4
                mm(banks[b][0:NSEQ, :], cS3[:, kt, :], STG[st][:, (kt % 4) * 512:(kt % 4 + 1) * 512],
                   kt == 0, kt == KT - 1, [c_stg[st], c_cS], [bcell[b]])
            rowb = rep[0:NSEQ, (j % 2) * 512:(j % 2 + 1) * 512]
            P.op("act", lambda h: h.copy(out=rowb, in_=banks[b][0:NSEQ, :]), reads=[bcell[b]], writes=[c_rep])
            for nt in range(4):
                col = (j * 4 + nt) * NSEQ
                P.op("pe", lambda h: h.transpose(out=banks[0][:, col:col + NSEQ], in_=rowb[:, nt * 128:(nt + 1) * 128],
                                                 identity=ident[0:NSEQ, 0:NSEQ]),
                     reads=[c_rep, c_const], writes=[bcell[0]])
        P.op("dve", lambda h: h.tensor_tensor(
            out=modT[:].rearrange("p (n s) -> p n s", s=NSEQ),
            in0=banks[0][:, 0:48 * NSEQ].rearrange("p (n s) -> p n s", s=NSEQ),
            in1=badaT[:].unsqueeze(2).to_broadcast([128, 48, NSEQ]), op=ALU.add),
            reads=[bcell[0], c_const], writes=[c_modT])
        P.barrier()
        maybe_stop('P0')

        modT3 = modT[:].rearrange("p (n s) -> p n s", s=NSEQ)
        gT3 = gT[:].rearrange("p (a k) -> p a k", a=3)
        cv3 = cvec[:].rearrange("p (a k) -> p a k", a=3)

        def mod_vec(j, s):
            return modT3[:, j * KT:(j + 1) * KT, s]

        def norm_mod_transpose(s, a_idx, sh_idx, g_idx, src_tile, src_cells, dst_hook):
            P.op("dve", lambda h: h.scalar_tensor_tensor(out=cv3[:, 0, :], in0=mod_vec(a_idx, s), scalar=1.0,
                                                         in1=gT3[:, g_idx, :], op0=ALU.add, op1=ALU.mult),
                 reads=[c_modT, c_const], writes=[c_cvec])
            bcast(cv3[:, 0, :], 0, (0, 1))
            bcast(mod_vec(sh_idx, s), 1, (2, 3))
            pairs = Rot([(0, 1), (2, 3)])
            hN = [T_f32[:, 0:D], T_f32[:, D:2 * D]]
            hNc = [[c_T[0], c_T[1]], [c_T[2], c_T[3]]]
            pre = {}

            def stats(i):
                xt, xc = src_tile(i)
                P.op("dve", lambda h: h.scalar_tensor_tensor(out=junk[:], in0=xt, scalar=1.0, in1=xt, op0=ALU.mult,
                                                             op1=ALU.mult, accum_out=ssq[:, i:i + 1]),
                     reads=xc, writes=[c_junk, c_ssq])
                rstd_from_ssq(i, 1, 1.0 / D)
                pre[i] = (xt, xc)

            stats(0)
            for i in range(NT):
                if i + 1 < NT:
                    stats(i + 1)
                xt, xc = pre.pop(i)
                hb = i % 2
                P.op("dve", lambda h: h.scalar_tensor_tensor(out=hN[hb], in0=xt, scalar=rstd[:, i:i + 1], in1=BC[0][:],
                                                             op0=ALU.mult, op1=ALU.mult),
                     reads=list(xc) + [c_rstd, c_BC[0]], writes=hNc[hb])
                P.op("dve", lambda h: h.tensor_tensor(out=hN[hb], in0=hN[hb], in1=BC[1][:], op=ALU.add),
                     reads=hNc[hb] + [c_BC[1]], writes=hNc[hb])
                ba, bb = pairs.next()
                for kt in range(KT):
                    b = ba if kt < 4 else bb
                    tr(banks[b][:, (kt % 4) * 128:(kt % 4 + 1) * 128], hN[hb][:, kt * 128:(kt + 1) * 128],
                       hNc[hb], [bcell[b]])
                dst_hook(i, ba, bb)

        def store_dbg(name, ap_sb, shape, dt, cells):
            t = nc.dram_tensor("dbg_" + name, list(shape), dt, kind="ExternalOutput").ap()
            dbg_d[name] = t
            P.dma("sp", t, ap_sb, reads=cells, key="dbg")

        for s in range(NSEQ):
            xl = [T_f32[:, 2 * D:3 * D], T_f32[:, 3 * D:4 * D]]
            xlc = [[c_T[4], c_T[5]], [c_T[6], c_T[7]]]

            def src_x(i):
                P.dma("sp", xl[i % 2], x_d[s, i * 128:(i + 1) * 128, :], writes=xlc[i % 2], key="xl%d" % (i % 2))
                return xl[i % 2], xlc[i % 2]

            def h1_hook(i, ba, bb):
                for half, b in enumerate((ba, bb)):
                    P.op("act", lambda h, b=b, half=half: h.copy(
                        out=YT[:, half * 4:(half + 1) * 4, i * 128:(i + 1) * 128],
                        in_=banks[b][:].rearrange("p (k t) -> p k t", k=4)),
                        reads=[bcell[b]], writes=[c_Y[half * 4 + k] for k in range(4)])

            norm_mod_transpose(s, 1, 0, 0, src_x, None, h1_hook)
            if dbg and s == 0:
                store_dbg("h1T", Y_bf[:], [128, KT * S], BF16, c_Y)
            maybe_stop('A1n')

            wbf = [W_bf[:, 0:4096].rearrange("p (k n) -> p k n", k=KT), W_bf[:, 4096:8192].rearrange("p (k n) -> p k n", k=KT)]
            wbc = [c_W[0], c_W[1]]
            prj = Rot([4, 5, 6, 7])
            trb = Rot([0, 1, 2, 3])
            qtm = [T_f32[:, 2 * D:2 * D + 512], T_f32[:, 2 * D + 512:3 * D]]
            qtmc = [c_T[4], c_T[5]]
            tmpr = T_f32[:, 3 * D:3 * D + 256]
            pend_tr = []
            for cch in range(4):
                wb = cch % 2
                for hf in range(2):
                    P.dma("sp", STG[hf][:].rearrange("p (k n) -> p k n", k=4),
                          win_d[hf * 512:(hf + 1) * 512, cch * 512:(cch + 1) * 512].rearrange("(k p) n -> p k n", p=128),
                          writes=[c_stg[hf]], key="stg%d" % hf)
                    P.op("pool", lambda h, hf=hf: h.tensor_copy(out=wbf[wb][:, hf * 4:(hf + 1) * 4, :],
                                                                in_=STG[hf][:].rearrange("p (k n) -> p k n", k=4)),
                         reads=[c_stg[hf]], writes=[wbc[wb]])
                for i in range(NT):
                    b = prj.next()
                    for kt in range(KT):
                        mm(banks[b][:], YT[:, kt, i * 128:(i + 1) * 128], wbf[wb][:, kt, :], kt == 0, kt == KT - 1,
                           [c_Y[kt], wbc[wb]], [bcell[b]])
                    while pend_tr:
                        pend_tr.pop(0)()
                    if cch < 2:
                        qb = i % 2
                        z3 = banks[b][:].rearrange("p (a d) -> p a d", a=8)
                        q3 = qtm[qb].rearrange("p (a d) -> p a d", a=8)
                        cosb = cosT[:, i, :].unsqueeze(1).to_broadcast([128, 8, 8])
                        sinb = sinT[:, i, :].unsqueeze(1).to_broadcast([128, 8, 8])
                        tA = tmpr[:, 0:64].rearrange("p (a d) -> p a d", a=8)
                        tB = tmpr[:, 64:128].rearrange("p (a d) -> p a d", a=8)
                        P.op("act", lambda h: h.copy(out=qtm[qb], in_=banks[b][:]), reads=[bcell[b]], writes=[qtmc[qb]])
                        P.op("dve", lambda h: h.tensor_tensor(out=tA, in0=z3[:, :, 0:8], in1=cosb, op=ALU.mult),
                             reads=[bcell[b], c_const], writes=[c_T[6]])
                        P.op("dve", lambda h: h.tensor_tensor(out=tB, in0=z3[:, :, 8:16], in1=sinb, op=ALU.mult),
                             reads=[bcell[b], c_const], writes=[c_T[6]])
                        P.op("dve", lambda h: h.tensor_tensor(out=q3[:, :, 0:8], in0=tA, in1=tB, op=ALU.subtract),
                             reads=[c_T[6]], writes=[qtmc[qb]])
                        P.op("dve", lambda h: h.tensor_tensor(out=tA, in0=z3[:, :, 8:16], in1=cosb, op=ALU.mult),
                             reads=[bcell[b], c_const], writes=[c_T[6]])
                        P.op("dve", lambda h: h.tensor_tensor(out=tB, in0=z3[:, :, 0:8], in1=sinb, op=ALU.mult),
                             reads=[bcell[b], c_const], writes=[c_T[6]])
                        P.op("dve", lambda h: h.tensor_tensor(out=q3[:, :, 8:16], in0=tA, in1=tB, op=ALU.add),
                             reads=[c_T[6]], writes=[qtmc[qb]])
                        def _tr_evac(qb=qb, i=i, cch=cch):
                            tb_ = trb.next()
                            for hh in range(NH):
                                tr(banks[tb_][:, hh * 128:(hh + 1) * 128], qtm[qb][:, hh * 128:(hh + 1) * 128],
                                   [qtmc[qb]], [bcell[tb_]])
                            dstv = qT_v() if cch == 0 else kT_v()
                            P.op("act", lambda h: h.copy(out=dstv[:, :, i * 128:(i + 1) * 128],
                                                         in_=banks[tb_][:].rearrange("p (k t) -> p k t", k=4)),
                                 reads=[bcell[tb_]], writes=(cQ if cch == 0 else cK))
                        pend_tr.append(_tr_evac)
                    else:
                        dstv = v_v() if cch == 2 else u_v()
                        P.op("act", lambda h: h.copy(out=dstv[:, i, :], in_=banks[b][:]),
                             reads=[bcell[b]], writes=(cV if cch == 2 else cU))
            while pend_tr:
                pend_tr.pop(0)()
            P.barrier()
            if dbg and s == 0:
                store_dbg("qkvu", X_bf[:], [128, 2 * NT * D], BF16, c_X)
                P.barrier()

            maybe_stop('A1')
            mixT = YT
            qT, kT, vv, uu = qT_v(), kT_v(), v_v(), u_v()
            srot = Rot([0, 1, 2, 3])
            Pb = [W_bf[:, 8192 + k * 512:8192 + (k + 1) * 512] for k in range(4)]
            Pc = [P.cell("P%d" % k) for k in range(4)]
            prot = Rot([0, 1, 2, 3])
            tA_, tB_, tR_, tO_ = (T_f32[:, k * 512:(k + 1) * 512] for k in range(4))
            cA, cB, cR, cO = c_T[0], c_T[1], c_T[2], c_T[3]
            tSq = T_f32[:, 2048:2560]
            cSq = c_T[4]
            steps = [(hh, qc, kt, comp) for hh in range(NH) for qc in range(NQC) for kt in range(NT) for comp in range(2)]
            LA = 2
            pk_of = {}
            deferred = []

            prot2 = Rot([0, 1])
            srot2 = Rot([0, 1])

            def emit_Spair(p_):
                hh, qc, kt, _ = steps[2 * p_]
                qs = slice(qc * 512, (qc + 1) * 512)
                ks = slice(kt * 128, (kt + 1) * 128)
                m_ = srot2.next()
                for comp in range(2):
                    ps = slice(comp * 64, (comp + 1) * 64)
                    sbk = 2 * m_ + comp
                    mm(banks[sbk][:], kT[ps, hh, ks], qT[ps, hh, qs], True, True, cQ + cK, [bcell[sbk]])
                pm = prot2.next()
                pk_of[2 * p_] = 2 * pm
                pk_of[2 * p_ + 1] = 2 * pm + 1
                P.op("act", lambda h: h.activation(out=W_bf[:, 8192 + 2 * pm * 512:8192 + (2 * pm + 2) * 512],
                                                   in_=bigb[m_][:, :], func=AF.Exp, scale=0.125),
                     reads=[bcell[2 * m_], bcell[2 * m_ + 1]], writes=[Pc[2 * pm], Pc[2 * pm + 1]])

            sL1 = T_f32[:, 2560:3072]
            sL2 = T_f32[:, 3072:3584]
            cL1, cL2 = c_T[5], c_T[6]

            def post1(hh, qc):
                P.op("act", lambda h: h.copy(out=tA_, in_=banks[4][:]), reads=[bcell[4]], writes=[cA])
                P.op("dve", lambda h: h.tensor_copy(out=sL1, in_=banks[5][:]), reads=[bcell[5]], writes=[cL1])
                P.op("act", lambda h: h.copy(out=tB_, in_=banks[6][:]), reads=[bcell[6]], writes=[cB])
                P.op("dve", lambda h: h.tensor_copy(out=sL2, in_=banks[7][:]), reads=[bcell[7]], writes=[cL2])
                P.op("dve", lambda h: h.reciprocal(out=sL1, in_=sL1), reads=[cL1], writes=[cL1])
                P.op("dve", lambda h: h.tensor_tensor(out=tA_, in0=tA_, in1=sL1, op=ALU.mult), reads=[cA, cL1], writes=[cA])
                P.op("dve", lambda h: h.reciprocal(out=sL2, in_=sL2), reads=[cL2], writes=[cL2])
                P.op("dve", lambda h: h.tensor_tensor(out=tB_, in0=tB_, in1=sL2, op=ALU.mult), reads=[cB, cL2], writes=[cB])
                P.op("dve", lambda h: h.scalar_tensor_tensor(out=tO_, in0=tB_, scalar=small[:, 3:4], in1=tA_,
                                                             op0=ALU.mult, op1=ALU.add),
                     reads=[cA, cB, c_small], writes=[cO])
                P.op("dve", lambda h: h.tensor_tensor(out=tSq, in0=tO_, in1=tO_, op=ALU.mult), reads=[cO], writes=[cSq])

            def post2(hh, qc):
                qs = slice(qc * 512, (qc + 1) * 512)
                sb2 = 2 * srot2.next()
                mm(banks[sb2][:], ones32[:], tSq, True, True, [c_const, cSq], [bcell[sb2]])
                P.op("act", lambda h: h.activation(out=tR_, in_=banks[sb2][:], func=AF.Ln, scale=1.0 / 128, bias=small[:, 2:3]),
                     reads=[bcell[sb2], c_small], writes=[cR])
                P.op("act", lambda h: h.activation(out=tR_, in_=tR_, func=AF.Exp, scale=-0.5), reads=[cR], writes=[cR])
                P.op("dve", lambda h: h.scalar_tensor_tensor(out=mixT[:, hh, qs], in0=tO_, scalar=small[:, 1:2], in1=tR_,
                                                             op0=ALU.mult, op1=ALU.mult),
                     reads=[cO, cR, c_small], writes=[c_Y[hh]])

            def emit_AV(idx):
                hh, qc, kt, comp = steps[idx]
                pk = pk_of.pop(idx)
                ob, lb = (4, 5) if comp == 0 else (6, 7)
                mm(banks[ob][:], vv[:, kt, hh * 128:(hh + 1) * 128], Pb[pk], kt == 0, kt == NT - 1,
                   cV + [Pc[pk]], [bcell[ob]])
                mm(banks[lb][:], ones_bf[:], Pb[pk], kt == 0, kt == NT - 1, [c_const, Pc[pk]], [bcell[lb]])
                if kt == NT - 1 and comp == 1:
                    post1(hh, qc)
                    deferred.append((idx + min(14, 2 * NT - 2), (hh, qc)))

            npairs = len(steps) // 2
            LAp = 1
            for p_ in range(npairs + LAp):
                if p_ < npairs:
                    emit_Spair(p_)
                j = p_ - LAp
                if j >= 0:
                    emit_AV(2 * j)
                    emit_AV(2 * j + 1)
                    while deferred and deferred[0][0] <= 2 * j + 1:
                        _, (h_, q_) = deferred.pop(0)
                        post2(h_, q_)
            while deferred:
                _, (h_, q_) = deferred.pop(0)
                post2(h_, q_)
            maybe_stop('A2')
            Ab4 = Ab_bf[:].rearrange("p (g v t) -> p g v t", g=NG, v=5)
            wp3 = wpool_bf[:].rearrange("p (g d) -> p g d", g=NG)
            dT = [W_bf[:, 8192 + 2048 + k * 512:8192 + 2048 + (k + 1) * 512] for k in range(2)]
            dTc = [P.cell("dT0"), P.cell("dT1")]
            for g in range(NG):
                for j in range(NQC):
                    b = srot.next()
                    for tt in range(4):
                        i = j * 4 + tt
                        rels = [r for r in (-1, 0, 1) if 0 <= i + r < NT]
                        for n_, r in enumerate(rels):
                            if r == 0:
                                var = 3 if i == 0 else (4 if i == NT - 1 else 1)
                            else:
                                var = 0 if r == -1 else 2
                            mm(banks[b][:, tt * 128:(tt + 1) * 128], uu[:, i + r, g * 128:(g + 1) * 128], Ab4[:, g, var, :],
                               n_ == 0, n_ == len(rels) - 1, cU + [c_const], [bcell[b]])
                    db = (g * NQC + j) % 2
                    P.op("act", lambda h: h.copy(out=dT[db], in_=banks[b][:]), reads=[bcell[b]], writes=[dTc[db]])
                    b2 = srot.next()
                    mm(banks[b2][:], wp3[:, g, :], dT[db], True, True, [c_const, dTc[db]], [bcell[b2]])
                    P.op("dve", lambda h: h.tensor_scalar(out=mixT[:, 4 + g, j * 512:(j + 1) * 512], in0=banks[b2][:],
                                                          scalar1=pscaleT[:, g:g + 1], scalar2=None, op0=ALU.mult),
                         reads=[bcell[b2], c_const], writes=[c_Y[4 + g]])
            P.barrier()
            if dbg and s == 0:
                store_dbg("mixT", Y_bf[:], [128, KT * S], BF16, c_Y)
                P.barrier()

            maybe_stop('A2b')
            for i in range(NT):
                P.dma("sp", xs_tile(i), x_d[s, i * 128:(i + 1) * 128, :], writes=[c_X[i]], key="xs%d" % i)
            bcast(mod_vec(2, s), 0, (0, 1))
            wo = W_bf[:, 0:8192].rearrange("p (k n) -> p k n", k=KT)
            for cc in range(2):
                for hf in range(2):
                    P.dma("sp", STG[hf][:].rearrange("p (k n) -> p k n", k=4),
                          wout_d[hf * 512:(hf + 1) * 512, cc * 512:(cc + 1) * 512].rearrange("(k p) n -> p k n", p=128),
                          writes=[c_stg[hf]], key="stg%d" % hf)
                    P.op("pool", lambda h, hf=hf, cc=cc: h.tensor_tensor(
                        out=wo[:, hf * 4:(hf + 1) * 4, cc * 512:(cc + 1) * 512],
                        in0=STG[hf][:].rearrange("p (k n) -> p k n", k=4),
                        in1=BC[0][:, cc * 512:(cc + 1) * 512].unsqueeze(1).to_broadcast([128, 4, 512]), op=ALU.mult),
                        reads=[c_stg[hf], c_BC[0]], writes=[c_W[0], c_W[1]])
            orot = Rot([2, 3, 4, 5, 6, 7])
            for i in range(NT):
                for cc in range(2):
                    b = orot.next()
                    for kt in range(KT):
                        mm(banks[b][:], mixT[:, kt, i * 128:(i + 1) * 128], wo[:, kt, cc * 512:(cc + 1) * 512],
                           kt == 0, kt == KT - 1, [c_Y[kt], c_W[0], c_W[1]], [bcell[b]])
                    xsl = xs_tile(i)[:, cc * 512:(cc + 1) * 512]
                    P.op("dve", lambda h: h.tensor_tensor(out=xsl, in0=banks[b][:], in1=xsl, op=ALU.add),
                         reads=[bcell[b], c_X[i]], writes=[c_X[i]])
            P.barrier()
            if dbg and s == 0:
                store_dbg("x1", X_f32[:], [128, NT * D], F32, c_X)
                P.barrier()

            maybe_stop('A3')
            h2T = YT
            h32 = [T_f32[:, 2 * D:3 * D].rearrange("p (k t) -> p k t", k=KT), T_f32[:, 3 * D:4 * D].rearrange("p (k t) -> p k t", k=KT)]
            h32c = [[c_T[4], c_T[5]], [c_T[6], c_T[7]]]
            wr3 = wr_sb[:].rearrange("p (k n) -> p k n", k=KT)
            rrot = Rot([4, 5, 6, 7])
            Wd3 = Wdn[:].rearrange("p (i e) -> p i e", e=NE)

            def src_x1(i):
                return xs_tile(i), [c_X[i]]

            def h2_hook(i, ba, bb):
                hb = i % 2
                for half, b in enumerate((ba, bb)):
                    P.op("act", lambda h, b=b, half=half: h.copy(
                        out=h32[hb][:, half * 4:(half + 1) * 4, :], in_=banks[b][:].rearrange("p (k t) -> p k t", k=4)),
                        reads=[bcell[b]], writes=h32c[hb])
                P.op("pool", lambda h: h.tensor_copy(out=h2T[:, :, i * 128:(i + 1) * 128], in_=h32[hb]),
                     reads=h32c[hb], writes=c_Y)
                def _router(i=i, hb=hb):
                    rb = rrot.next()
                    for kt in range(KT):
                        mm(banks[rb][:, 0:36], h32[hb][:, kt, :], wr3[:, kt, :], kt == 0, False, h32c[hb] + [c_const], [bcell[rb]])
                    mm(banks[rb][:, 0:36], ones32[0:1, :], br_sb[0:1, :], False, True, [c_const], [bcell[rb]])

                    def _copies():
                        P.op("dve", lambda h: h.tensor_copy(out=GLb[:, i * NG:(i + 1) * NG], in_=banks[rb][:, 0:4]),
                             reads=[bcell[rb]], writes=[c_lg])
                        P.op("dve", lambda h: h.tensor_copy(out=ELb[:, i * NE:(i + 1) * NE], in_=banks[rb][:, 4:36]),
                             reads=[bcell[rb]], writes=[c_lg])
                    pend_cp.append(_copies)
                if len(pend_cp) >= 2:
                    pend_cp.pop(0)()
                while pend_rt:
                    pend_rt.pop(0)()
                pend_rt.append(_router)

            pend_rt = []
            pend_cp = []
            norm_mod_transpose(s, 4, 3, 1, src_x1, None, h2_hook)
            while pend_rt:
                pend_rt.pop(0)()
            while pend_cp:
                pend_cp.pop(0)()
            _o = [0]

            def TV(n):
                a_ = _o[0]
                _o[0] += n
                return T_f32[:, a_:a_ + n]

            RC = c_T
            GL3 = GLb[:].rearrange("p (i g) -> p i g", g=NG)
            EL3 = ELb[:].rearrange("p (a j) -> p a j", j=EPG)
            gmax = TV(NT); goh = TV(NT * NG); gsh = TV(NT * NG); gsum = TV(NT); gp = TV(NT)
            esel = TV(NT * NE); ein = TV(NT * EPG); m1 = TV(NT); mk1 = TV(NT * EPG); ein2 = TV(NT * EPG)
            m2 = TV(NT); mk2 = TV(NT * EPG); dm = TV(NT); ex = TV(NT); den = TV(NT); p1 = TV(NT); p2 = TV(NT)
            w1 = TV(NT); w2 = TV(NT); we = TV(NT * EPG); tm1 = TV(NT * EPG); tm2 = TV(NT * EPG)
            g3 = lambda ap: ap.rearrange("p (i g) -> p i g", g=NG)
            j3 = lambda ap: ap.rearrange("p (i j) -> p i j", j=EPG)
            bc = lambda ap, n: ap.unsqueeze(2).to_broadcast([128, NT, n])
            RD = [c_lg] + RC

            def dv(fn):
                P.op("dve", fn, reads=RD, writes=RC)

            dv(lambda h: h.tensor_reduce(out=gmax, in_=GL3, axis=AX.X, op=ALU.max))
            dv(lambda h: h.tensor_tensor(out=g3(goh), in0=GL3, in1=bc(gmax, NG), op=ALU.is_equal))
            dv(lambda h: h.tensor_tensor(out=g3(gsh), in0=GL3, in1=bc(gmax, NG), op=ALU.subtract))
            P.op("act", lambda h: h.activation(out=gsh, in_=gsh, func=AF.Exp), reads=RC, writes=RC)
            dv(lambda h: h.tensor_reduce(out=gsum, in_=g3(gsh), axis=AX.X, op=ALU.add))
            dv(lambda h: h.reciprocal(out=gp, in_=gsum))
            dv(lambda h: h.tensor_tensor(out=esel.rearrange("p (a j) -> p a j", j=EPG), in0=EL3,
                                         in1=goh.unsqueeze(2).to_broadcast([128, NT * NG, EPG]), op=ALU.mult))
            dv(lambda h: h.tensor_reduce(out=j3(ein), in_=esel.rearrange("p (i g j) -> p i j g", g=NG, j=EPG),
                                         axis=AX.X, op=ALU.add))
            dv(lambda h: h.tensor_reduce(out=m1, in_=j3(ein), axis=AX.X, op=ALU.max))
            dv(lambda h: h.tensor_tensor(out=j3(mk1), in0=j3(ein), in1=bc(m1, EPG), op=ALU.is_equal))
            dv(lambda h: h.scalar_tensor_tensor(out=ein2, in0=mk1, scalar=-1e30, in1=ein, op0=ALU.mult, op1=ALU.add))
            dv(lambda h: h.tensor_reduce(out=m2, in_=j3(ein2), axis=AX.X, op=ALU.max))
            dv(lambda h: h.tensor_tensor(out=j3(mk2), in0=j3(ein2), in1=bc(m2, EPG), op=ALU.is_equal))
            dv(lambda h: h.tensor_tensor(out=dm, in0=m2, in1=m1, op=ALU.subtract))
            P.op("act", lambda h: h.activation(out=ex, in_=dm, func=AF.Exp), reads=RC, writes=RC)
            dv(lambda h: h.tensor_scalar(out=den, in0=ex, scalar1=1.0, scalar2=None, op0=ALU.add))
            dv(lambda h: h.reciprocal(out=p1, in_=den))
            dv(lambda h: h.tensor_tensor(out=p2, in0=ex, in1=p1, op=ALU.mult))
            dv(lambda h: h.tensor_tensor(out=w1, in0=p1, in1=gp, op=ALU.mult))
            dv(lambda h: h.tensor_tensor(out=w2, in0=p2, in1=gp, op=ALU.mult))
            dv(lambda h: h.tensor_tensor(out=j3(tm1), in0=j3(mk1), in1=bc(w1, EPG), op=ALU.mult))
            dv(lambda h: h.tensor_tensor(out=j3(tm2), in0=j3(mk2), in1=bc(w2, EPG), op=ALU.mult))
            dv(lambda h: h.tensor_tensor(out=we, in0=tm1, in1=tm2, op=ALU.add))
            for g in range(NG):
                P.op("dve", lambda h: h.tensor_tensor(out=Wd3[:, :, g * EPG:(g + 1) * EPG], in0=j3(we),
                                                      in1=g3(goh)[:, :, g:g + 1].to_broadcast([128, NT, EPG]), op=ALU.mult),
                     reads=RC, writes=[c_Wdn])
            P.barrier()
            if dbg and s == 0:
                store_dbg("Wdn", Wdn[:], [128, NT * NE], F32, [c_Wdn])
                store_dbg("h2T", Y_bf[:], [128, KT * S], BF16, c_Y)
                P.barrier()

            maybe_stop('B1')
            bcast(mod_vec(5, s), 0, (0, 1))
            wslot = [(W_bf[:, sl * 12288:sl * 12288 + 4096].rearrange("p (k n) -> p k n", k=KT),
                      W_bf[:, sl * 12288 + 4096:sl * 12288 + 8192].rearrange("p (k n) -> p k n", k=KT),
                      W_bf[:, sl * 12288 + 8192:sl * 12288 + 12288].rearrange("p (f n) -> p f n", f=4)) for sl in range(2)]
            wcell = [[c_W[0], c_W[1], c_W[2]], [c_W[3], c_W[4], c_W[5]]]
            hid = [T_f32[:, 0:1024].bitcast(BF16).rearrange("p (f t) -> p f t", f=4),
                   T_f32[:, 1024:2048].bitcast(BF16).rearrange("p (f t) -> p f t", f=4)]
            hidc = [[c_T[0], c_T[1]], [c_T[2], c_T[3]]]
            sg = [T_f32[:, 2048:2560], T_f32[:, 2560:3072]]
            sgc = [c_T[4], c_T[5]]
            gurot = Rot([(0, 1), (2, 3)])
            yrot = Rot([4, 5, 6, 7])
            sgrot = Rot([0, 1])
            stg_i = [0]

            def stage_next():
                k = stg_i[0] % 2
                stg_i[0] += 1
                return k

            def load_expert(e):
                sl = e % 2
                wgv, wuv, wdv = wslot[sl]
                for which, (src, dstv) in enumerate(((wg_d, wgv), (wu_d, wuv))):
                    for hf in range(2):
                        st = stage_next()
                        P.dma("sp", STG[st][:].rearrange("p (k n) -> p k n", k=4),
                              src[e, hf * 512:(hf + 1) * 512, :].rearrange("(k p) n -> p k n", p=128),
                              writes=[c_stg[st]], key="stg%d" % st)
                        P.op("pool", lambda h, st=st, dstv=dstv, hf=hf: h.tensor_copy(
                            out=dstv[:, hf * 4:(hf + 1) * 4, :], in_=STG[st][:].rearrange("p (k n) -> p k n", k=4)),
                            reads=[c_stg[st]], writes=[wcell[sl][which]])
                for hf in range(2):
                    st = stage_next()
                    P.dma("sp", STG[st][:].rearrange("p (f n) -> p f n", f=2),
                          wd_d[e, hf * 256:(hf + 1) * 256, :].rearrange("(f p) n -> p f n", p=128),
                          writes=[c_stg[st]], key="stg%d" % st)
                    P.op("pool", lambda h, st=st, hf=hf: h.tensor_tensor(
                        out=wdv[:, hf * 2:(hf + 1) * 2, :], in0=STG[st][:].rearrange("p (f n) -> p f n", f=2),
                        in1=BC[0][:].unsqueeze(1).to_broadcast([128, 2, D]), op=ALU.mult),
                        reads=[c_stg[st], c_BC[0]], writes=[wcell[sl][2]])

            def gate_up(e, tc):
                sl = e % 2
                wgv, wuv, _ = wslot[sl]
                hb = tc % 2
                ts_ = slice(tc * 512, (tc + 1) * 512)
                for f in range(4):
                    gb, ub = gurot.next()
                    for kt in range(KT):
                        mm(banks[gb][:], wgv[:, kt, f * 128:(f + 1) * 128], h2T[:, kt, ts_], kt == 0, kt == KT - 1,
                           [wcell[sl][0], c_Y[kt]], [bcell[gb]])
                    for kt in range(KT):
                        mm(banks[ub][:], wuv[:, kt, f * 128:(f + 1) * 128], h2T[:, kt, ts_], kt == 0, kt == KT - 1,
                           [wcell[sl][1], c_Y[kt]], [bcell[ub]])
                    si = sgrot.next()
                    P.op("act", lambda h, gb=gb, si=si: h.activation(out=sg[si], in_=banks[gb][:], func=AF.Silu),
                         reads=[bcell[gb]], writes=[sgc[si]])
                    P.op("dve", lambda h, ub=ub, si=si, f=f: h.tensor_tensor(out=hid[hb][:, f, :], in0=banks[ub][:], in1=sg[si], op=ALU.mult),
                         reads=[bcell[ub], sgc[si]], writes=hidc[hb])

            def down(e, tc):
                sl = e % 2
                wdv = wslot[sl][2]
                hb = tc % 2
                for tt in range(4):
                    i = tc * 4 + tt
                    for cc in range(2):
                        yb = yrot.next()
                        for f in range(4):
                            mm(banks[yb][:], hid[hb][:, f, tt * 128:(tt + 1) * 128], wdv[:, f, cc * 512:(cc + 1) * 512],
                               f == 0, f == 3, hidc[hb] + [wcell[sl][2]], [bcell[yb]])
                        xsl = xs_tile(i)[:, cc * 512:(cc + 1) * 512]
                        P.op("dve", lambda h, yb=yb, xsl=xsl, i=i: h.scalar_tensor_tensor(
                            out=xsl, in0=banks[yb][:], scalar=Wd3[:, i, e:e + 1], in1=xsl, op0=ALU.mult, op1=ALU.add),
                            reads=[bcell[yb], c_Wdn, c_X[i]], writes=[c_X[i]])

            load_expert(0)
            for e in range(n_exp):
                if e + 1 < n_exp:
                    load_expert(e + 1)
                gate_up(e, 0)
                for tc in range(NQC):
                    if tc + 1 < NQC:
                        gate_up(e, tc + 1)
                    down(e, tc)
            P.barrier()

            maybe_stop('B2')
            bcast(gT3[:, 2, :], 1, (0, 1))
            for i in range(NT):
                xt = xs_tile(i)
                P.op("dve", lambda h: h.scalar_tensor_tensor(out=junk[:], in0=xt, scalar=1.0, in1=xt, op0=ALU.mult,
                                                             op1=ALU.mult, accum_out=ssq[:, NT + i:NT + i + 1]),
                     reads=[c_X[i]], writes=[c_junk, c_ssq])
                rstd_from_ssq(NT + i, 1, 1.0 / D)
                P.op("dve", lambda h: h.scalar_tensor_tensor(out=xt, in0=xt, scalar=rstd[:, NT + i:NT + i + 1], in1=BC[1][:],
                                                             op0=ALU.mult, op1=ALU.mult),
                     reads=[c_X[i], c_rstd, c_BC[1]], writes=[c_X[i]])
                P.dma("sp", out_d[s, i * 128:(i + 1) * 128, :], xt, reads=[c_X[i]], key="xs%d" % i)
            P.barrier()
    except _Stop:
        pass
    return nc, dbg_d, P


def _prep_shared(S, inputs):
    f = lambda a: np.ascontiguousarray(np.asarray(a, dtype=np.float32))
    cos, sin = _rope_tables(S)
    NT = S // 128
    sh = {
        "w_ada": f(inputs["w_ada"][0]),
        "b_adaT": f(np.asarray(inputs["b_ada"][0]).reshape(N_MOD * KT, 128).T),
        "gT": f(np.stack([np.asarray(inputs["norm1_g"][0]).reshape(KT, 128).T,
                          np.asarray(inputs["norm2_g"][0]).reshape(KT, 128).T,
                          np.asarray(inputs["final_g"]).reshape(KT, 128).T], axis=1)),
        "w_in": f(inputs["w_in"][0]),
        "lam4": f(np.stack([np.asarray(inputs["lambda_q1"][0]), np.asarray(inputs["lambda_k1"][0]),
                            np.asarray(inputs["lambda_q2"][0]), np.asarray(inputs["lambda_k2"][0])], axis=0)),
        "subln": f(np.asarray(inputs["subln_g"][0]).reshape(128, 1)),
        "w_pool": f(inputs["w_pool"][0]),
        "pscaleT": f(np.asarray(inputs["pool_scale"][0]).T),
        "w_out": f(inputs["w_out"][0]),
        "w_r": f(np.concatenate([np.asarray(inputs["w_router_group"][0]), np.asarray(inputs["w_router_expert"][0])], axis=1)),
        "b_r": f(np.concatenate([np.asarray(inputs["b_router_group"][0]), np.asarray(inputs["b_router_expert"][0])]).reshape(1, 36)),
        "w_gate": f(inputs["w_gate"][0]),
        "w_up": f(inputs["w_up"][0]),
        "w_down": f(inputs["w_down"][0]),
        "ident": np.eye(128, dtype=np.float32),
        "cosT": f(cos.reshape(NT, 128, 8).transpose(1, 0, 2)),
        "sinT": f(sin.reshape(NT, 128, 8).transpose(1, 0, 2)),
        "Ab": _pool_blocks(S),
    }
    return sh


_CACHE = {}


def run(inputs, dbg=False, n_exp=NE):
    x = np.asarray(inputs["x"], dtype=np.float32)
    c = np.asarray(inputs["c"], dtype=np.float32)
    B, S, _ = x.shape
    NSEQ = B // NCORES
    key = (S, NSEQ, n_exp, dbg)
    if key not in _CACHE:
        _CACHE[key] = build_nc(S, NSEQ, n_exp=n_exp, dbg=dbg)
    nc, dbg_d, P = _CACHE[key]
    sh = _prep_shared(S, inputs)
    in_maps = []
    for core in range(NCORES):
        m = dict(sh)
        m["x"] = np.ascontiguousarray(x[core * NSEQ:(core + 1) * NSEQ])
        cc = c[core * NSEQ:(core + 1) * NSEQ]
        m["c_t"] = np.ascontiguousarray(cc.reshape(NSEQ, KT, 128).transpose(2, 1, 0))
        in_maps.append(m)
    res = run_bass_kernel_spmd(nc, in_maps, core_ids=list(range(NCORES)))
    out = np.concatenate([r["out"] for r in res.results], axis=0)
    if dbg:
        return out, res.results
    return out


def kernel(**inputs):
    return run(inputs).astype(np.float32)
```
